# Optimizing a Trainium2 kernel written in Bass

```python
import jax, jax.numpy as jnp
from jax import lax
import numpy as np

D_MODEL = 1024
BATCH = 8
SEQ = 4096
DEPTH = 4

MIX_WIDTH = D_MODEL
HG_WIDTH = MIX_WIDTH // 2
LRU_WIDTH = MIX_WIDTH - HG_WIDTH
HG_HEADS = 4
HG_HEAD_DIM = HG_WIDTH // HG_HEADS
HG_CHUNK = 32
LB_FLOOR = 1e-30
LRU_HEADS = 8
LRU_HEAD_DIM = LRU_WIDTH // LRU_HEADS
LRU_C = 8.0
CONV_WIDTH = 4
CONV_PAD = (2, 1)
N_EXPERTS = 32
TOP_K = 4
D_EXPERT = D_MODEL
SWIGLU_LIMIT = 7.0
SWIGLU_ALPHA = 1.702
EXPERT_BLOCK = 256
DN_ALPHA = float((2 * DEPTH) ** 0.25)
DN_BETA = float((8 * DEPTH) ** -0.25)
LN_EPS = 1e-5
RMS_EPS = 1e-6
IN_COLS = 5 * HG_WIDTH + 2 * LRU_WIDTH

kernel_name = "hybrid_hgrn2_rglru_moe_deepnorm_encoder"


def _layernorm(t, g, b):
    tf = t.astype(jnp.float32)
    mu = jnp.mean(tf, axis=-1, keepdims=True)
    var = jnp.mean(jnp.square(tf - mu), axis=-1, keepdims=True)
    y = (tf - mu) * lax.rsqrt(var + LN_EPS) * g.astype(jnp.float32) + b.astype(jnp.float32)
    return y.astype(t.dtype)


def _rmsnorm(t, g):
    return t * lax.rsqrt(jnp.mean(jnp.square(t), axis=-1, keepdims=True) + RMS_EPS) * g


def _hgrn2_chunked(q, k, v, logf):
    b, sl, h, dk = q.shape
    dv = v.shape[-1]
    n = sl // HG_CHUNK
    blk = lambda t: t.reshape(b, n, HG_CHUNK, h, t.shape[-1])
    q, k, v, logf = blk(q), blk(k), blk(v), blk(logf)
    g = jnp.cumsum(logf, axis=2)
    g_ref = g[:, :, HG_CHUNK // 2 - 1:HG_CHUNK // 2]
    g_last = g[:, :, -1:]
    tri = jnp.tril(jnp.ones((HG_CHUNK, HG_CHUNK), dtype=bool))
    scores = jnp.einsum('bnthd,bnshd->bnhts', q * jnp.exp(g - g_ref), k * jnp.exp(g_ref - g))
    scores = jnp.where(tri, scores, 0.0)
    o_intra = jnp.einsum('bnhts,bnshv->bnthv', scores, v)
    chunk_kv = jnp.einsum('bnshd,bnshv->nbhdv', k * jnp.exp(g_last - g), v)
    chunk_decay = jnp.moveaxis(jnp.exp(g_last[:, :, 0]), 1, 0)

    def step(state, inp):
        dec, kv = inp
        return state * dec[..., None] + kv, state

    _, s_in = lax.scan(step, jnp.zeros((b, h, dk, dv), q.dtype), (chunk_decay, chunk_kv))
    o_inter = jnp.einsum('bnthd,nbhdv->bnthv', q * jnp.exp(g), s_in)
    return (o_intra + o_inter).reshape(b, sl, h, dv)


def _hgrn2_mixer(zq, zi, zf, zb, zg, lb, norm_g):
    b, sl, _ = zq.shape
    heads = lambda t: t.reshape(b, sl, HG_HEADS, HG_HEAD_DIM)
    q = heads(jax.nn.silu(zq.astype(jnp.float32)))
    v = heads(zi.astype(jnp.float32))

    def gates(z, lb_d):
        z = z.astype(jnp.float32)
        log_lb = jnp.log(jnp.maximum(lb_d, LB_FLOOR))
        logf = jnp.logaddexp(log_lb, jnp.log1p(-lb_d) + jax.nn.log_sigmoid(z))
        k = (1.0 - lb_d) * jax.nn.sigmoid(-z)
        return heads(k), heads(logf)

    k_f, logf_f = gates(zf, lb[0])
    k_b, logf_b = gates(zb, lb[1])
    o_f = _hgrn2_chunked(q, k_f, v, logf_f)
    rev = lambda t: jnp.flip(t, axis=1)
    o_b = rev(_hgrn2_chunked(rev(q), rev(k_b), rev(v), rev(logf_b)))
    o = _rmsnorm(o_f + o_b, norm_g.astype(jnp.float32).reshape(HG_HEADS, HG_HEAD_DIM))
    o = o.reshape(b, sl, HG_WIDTH) * jax.nn.silu(zg.astype(jnp.float32))
    return o


def _rg_lru(xc, wa, ba, wx, bx, lam, reverse):
    b, sl, w = xc.shape
    xh = xc.reshape(b, sl, LRU_HEADS, LRU_HEAD_DIM)
    r = jax.nn.sigmoid(jnp.einsum('bshi,hij->bshj', xh, wa.astype(jnp.float32)).reshape(b, sl, w)
                       + ba.astype(jnp.float32))
    i = jax.nn.sigmoid(jnp.einsum('bshi,hij->bshj', xh, wx.astype(jnp.float32)).reshape(b, sl, w)
                       + bx.astype(jnp.float32))
    log_a = -LRU_C * r * jax.nn.softplus(-lam.astype(jnp.float32))
    a = jnp.exp(log_a)
    u = jnp.sqrt(jnp.maximum(-jnp.expm1(2.0 * log_a), 0.0)) * (i * xc)

    def combine(e1, e2):
        a1, b1 = e1
        a2, b2 = e2
        return a1 * a2, a2 * b1 + b2

    _, h = lax.associative_scan(combine, (a, u), axis=1, reverse=reverse)
    return h


def _griffin_mixer(zx, zy, conv_w, conv_b, wa, ba, wx, bx, lam, norm_g):
    xc = lax.conv_general_dilated(zx, conv_w[:, None, :], window_strides=(1,), padding=[CONV_PAD],
                                  dimension_numbers=('NWC', 'WIO', 'NWC'),
                                  feature_group_count=LRU_WIDTH)
    xc = (xc + conv_b).astype(jnp.float32)
    h = (_rg_lru(xc, wa[0], ba[0], wx[0], bx[0], lam[0], False)
         + _rg_lru(xc, wa[1], ba[1], wx[1], bx[1], lam[1], True))
    return _rmsnorm(h, norm_g.astype(jnp.float32)) * jax.nn.gelu(zy.astype(jnp.float32))


def _moe(x2, router_w, router_b, w_gu, b_gu, w_dn, b_dn):
    t, d = x2.shape
    dt = x2.dtype
    logits = jnp.einsum('td,de->te', x2, router_w).astype(jnp.float32) + router_b.astype(jnp.float32)
    top_logits, top_idx = lax.top_k(logits, TOP_K)
    gates = jax.nn.softmax(top_logits, axis=-1)
    n_assign = t * TOP_K
    flat_e = top_idx.reshape(-1).astype(jnp.int32)
    flat_tok = jnp.arange(n_assign, dtype=jnp.int32) // TOP_K
    flat_w = gates.reshape(-1)
    order = jnp.argsort(flat_e)
    s_e, s_tok, s_w = flat_e[order], flat_tok[order], flat_w[order]
    counts = jnp.bincount(flat_e, length=N_EXPERTS).astype(jnp.int32)
    starts = jnp.cumsum(counts) - counts
    padded = ((counts + EXPERT_BLOCK - 1) // EXPERT_BLOCK) * EXPERT_BLOCK
    pends = jnp.cumsum(padded)
    pstarts = pends - padded
    dest = pstarts[s_e] + jnp.arange(n_assign, dtype=jnp.int32) - starts[s_e]
    n_rows = ((n_assign + EXPERT_BLOCK - 1) // EXPERT_BLOCK) * EXPERT_BLOCK + N_EXPERTS * EXPERT_BLOCK
    n_blocks = n_rows // EXPERT_BLOCK
    row_tok = jnp.zeros((n_rows,), jnp.int32).at[dest].set(s_tok).reshape(n_blocks, EXPERT_BLOCK)
    row_w = jnp.zeros((n_rows,), dt).at[dest].set(s_w.astype(dt)).reshape(n_blocks, EXPERT_BLOCK)
    blk_start = jnp.arange(n_blocks, dtype=jnp.int32) * EXPERT_BLOCK
    blk_e = jnp.minimum(jnp.searchsorted(pends, blk_start, side='right'), N_EXPERTS - 1).astype(jnp.int32)

    def body(out, inp):
        tok, wgt, e = inp
        xb = x2[tok]
        gu = xb @ w_gu[e] + b_gu[e]
        gate = jnp.minimum(gu[:, :D_EXPERT], SWIGLU_LIMIT)
        up = jnp.clip(gu[:, D_EXPERT:], -SWIGLU_LIMIT, SWIGLU_LIMIT)
        hid = (up + 1.0) * (gate * jax.nn.sigmoid(SWIGLU_ALPHA * gate))
        y = hid @ w_dn[e] + b_dn[e]
        return out.at[tok].add(y * wgt[:, None]), None

    out, _ = lax.scan(body, jnp.zeros_like(x2), (row_tok, row_w, blk_e))
    return out


def setup_inputs(seed: int = 0) -> dict:
    key = jax.random.key(seed)
    ks = jax.random.split(key, 24)
    f32 = jnp.float32
    L = DEPTH
    nrm = lambda k, shape, scale: jax.random.normal(k, shape, f32) * scale
    a0 = jax.random.uniform(ks[10], (L, 2, LRU_WIDTH), f32, 0.9, 0.999)
    return {
        "x": nrm(ks[0], (BATCH, SEQ, D_MODEL), 1.0),
        "w_in": nrm(ks[1], (L, D_MODEL, IN_COLS), D_MODEL ** -0.5),
        "hg_lb": nrm(ks[2], (L, 2, HG_WIDTH), 0.5),
        "hg_norm": 1.0 + nrm(ks[3], (L, HG_WIDTH), 0.02),
        "lru_conv_w": nrm(ks[4], (L, CONV_WIDTH, LRU_WIDTH), CONV_WIDTH ** -0.5),
        "lru_conv_b": nrm(ks[5], (L, LRU_WIDTH), 0.02),
        "lru_wa": nrm(ks[6], (L, 2, LRU_HEADS, LRU_HEAD_DIM, LRU_HEAD_DIM), LRU_HEAD_DIM ** -0.5),
        "lru_ba": nrm(ks[7], (L, 2, LRU_WIDTH), 0.02),
        "lru_wx": nrm(ks[8], (L, 2, LRU_HEADS, LRU_HEAD_DIM, LRU_HEAD_DIM), LRU_HEAD_DIM ** -0.5),
        "lru_bx": nrm(ks[9], (L, 2, LRU_WIDTH), 0.02),
        "lru_lambda": jnp.log(a0) - jnp.log1p(-a0),
        "lru_norm": 1.0 + nrm(ks[11], (L, LRU_WIDTH), 0.02),
        "w_out": nrm(ks[12], (L, MIX_WIDTH, D_MODEL), MIX_WIDTH ** -0.5 * DN_BETA),
        "ln1_g": 1.0 + nrm(ks[13], (L, D_MODEL), 0.02),
        "ln1_b": nrm(ks[14], (L, D_MODEL), 0.02),
        "router_w": nrm(ks[15], (L, D_MODEL, N_EXPERTS), D_MODEL ** -0.5),
        "router_b": nrm(ks[16], (L, N_EXPERTS), 0.01),
        "w_gate_up": nrm(ks[17], (L, N_EXPERTS, D_MODEL, 2 * D_EXPERT), D_MODEL ** -0.5),
        "b_gate_up": nrm(ks[18], (L, N_EXPERTS, 2 * D_EXPERT), 0.02),
        "w_down": nrm(ks[19], (L, N_EXPERTS, D_EXPERT, D_MODEL), D_EXPERT ** -0.5 * DN_BETA),
        "b_down": nrm(ks[20], (L, N_EXPERTS, D_MODEL), 0.02),
        "ln2_g": 1.0 + nrm(ks[21], (L, D_MODEL), 0.02),
        "ln2_b": nrm(ks[22], (L, D_MODEL), 0.02),
    }


def reference(x, w_in, hg_lb, hg_norm, lru_conv_w, lru_conv_b, lru_wa, lru_ba, lru_wx, lru_bx,
              lru_lambda, lru_norm, w_out, ln1_g, ln1_b, router_w, router_b, w_gate_up, b_gate_up,
              w_down, b_down, ln2_g, ln2_b):
    dt = x.dtype
    b, sl, d = x.shape
    p = jax.nn.softmax(hg_lb.astype(jnp.float32), axis=0)
    lower_bounds = jnp.clip(jnp.cumsum(p, axis=0) - p[0:1], 0.0, 1.0 - 1e-6)
    split_at = [HG_WIDTH * j for j in range(1, 6)] + [5 * HG_WIDTH + LRU_WIDTH]
    for l in range(DEPTH):
        proj = jnp.einsum('bsd,dc->bsc', x, w_in[l])
        zq, zi, zf, zb, zg, zx, zy = jnp.split(proj, split_at, axis=-1)
        o_hg = _hgrn2_mixer(zq, zi, zf, zb, zg, lower_bounds[l], hg_norm[l])
        o_lru = _griffin_mixer(zx, zy, lru_conv_w[l], lru_conv_b[l], lru_wa[l], lru_ba[l],
                               lru_wx[l], lru_bx[l], lru_lambda[l], lru_norm[l])
        mix = jnp.concatenate([o_hg, o_lru], axis=-1).astype(dt)
        y = jnp.einsum('bsc,cd->bsd', mix, w_out[l])
        x = _layernorm(DN_ALPHA * x + y, ln1_g[l], ln1_b[l])
        m = _moe(x.reshape(b * sl, d), router_w[l], router_b[l], w_gate_up[l], b_gate_up[l],
                 w_down[l], b_down[l]).reshape(b, sl, d)
        x = _layernorm(DN_ALPHA * x + m, ln2_g[l], ln2_b[l])
    return x
```

```python
from contextlib import ExitStack
import numpy as np
import concourse.bass as bass
import concourse.mybir as mybir
from concourse.bass_utils import run_bass_kernel_spmd

F32 = mybir.dt.float32
BF16 = mybir.dt.bfloat16
I32 = mybir.dt.int32
ALU = mybir.AluOpType
AF = mybir.ActivationFunctionType
AX = mybir.AxisListType

D = 1024
NE = 32
DN_ALPHA = float(8 ** 0.25)
LN_EPS = 1e-5
RMS_EPS = 1e-6
import os
SAME_ENGINE_SYNC = set(os.environ.get('K_SES', 'dve,pool,sp').split(','))
NCON = 128 + 128 + 128 + 1 + 64 + 32 + 128 + 1


class Prog:
    ENG = ('pe', 'act', 'dve', 'pool', 'sp')

    def __init__(self, nc, es):
        self.nc = nc
        self.es = es
        self.E = {'pe': nc.tensor, 'act': nc.scalar, 'dve': nc.vector, 'pool': nc.gpsimd, 'sp': nc.sync}
        self.sem = {e: es.enter_context(nc.semaphore("c_" + e)) for e in self.ENG}
        self.cnt = {e: 0 for e in self.ENG}
        self.known = {e: {} for e in self.ENG}
        self.bufs = {}
        self.lanes = {}
        self.semname = {}
        for e in self.ENG:
            self.semname[id(self.sem[e])] = e

    def lane(self, name):
        if name not in self.lanes:
            s = self.es.enter_context(self.nc.semaphore("l_%d" % len(self.lanes)))
            self.lanes[name] = [s, 0]
        return self.lanes[name]

    BIG = set(os.environ.get('K_BIG', 'tg,ts,tu,hid,yt,acc,x1,x1t,kk,E,A,Bk,Kh,q,v,sg,xc,zx,xcb,gy,t1,t2').split(','))

    def _deps(self, eng, r, w, skip_sem=None):
        deps = {}
        def add(ev, b=None):
            if ev is None:
                return
            s, v = ev
            if s is skip_sem:
                return
            if s is self.sem[eng] and eng not in SAME_ENGINE_SYNC:
                return
            if s is self.sem[eng] and eng == 'dve' and b is not None:
                nm = b[0] if isinstance(b, tuple) else b
                if nm in self.BIG:
                    return
            k = id(s)
            if k not in deps or deps[k][1] < v:
                deps[k] = (s, v)
        for b in r:
            st = self.bufs.get(b)
            if st:
                add(st[0], b)
        for b in w:
            st = self.bufs.get(b)
            if st:
                add(st[0], b)
                for ev in st[1]:
                    add(ev, b)
        kn = self.known[eng]
        for k, (s, v) in deps.items():
            if kn.get(k, 0) >= v:
                continue
            self.E[eng].wait_ge(s, v)
            kn[k] = v

    def _mark(self, ev, r, w):
        for b in r:
            st = self.bufs.setdefault(b, [None, []])
            st[1].append(ev)
            if len(st[1]) > 24:
                best = {}
                for s, v in st[1]:
                    if id(s) not in best or best[id(s)][1] < v:
                        best[id(s)] = (s, v)
                st[1] = list(best.values())
        for b in w:
            self.bufs[b] = [ev, []]

    def op(self, eng, fn, r=(), w=()):
        self._deps(eng, r, w)
        ins = fn(self.E[eng])
        self.cnt[eng] += 1
        ins.then_inc(self.sem[eng], 1)
        self._mark((self.sem[eng], self.cnt[eng]), r, w)

    def dma(self, q, fn, r, w, lane):
        ln = self.lane(lane)
        self._deps(q, r, w, skip_sem=ln[0])
        ins = fn(self.E[q])
        ln[1] += 16
        ins.then_inc(ln[0], 16)
        self._mark((ln[0], ln[1]), r, w)

    def barrier(self):
        for e in self.ENG:
            kn = self.known[e]
            for e2 in self.ENG:
                if e2 == e or self.cnt[e2] == 0:
                    continue
                s = self.sem[e2]
                if kn.get(id(s), 0) < self.cnt[e2]:
                    self.E[e].wait_ge(s, self.cnt[e2])
                    kn[id(s)] = self.cnt[e2]
            for name, (s, v) in self.lanes.items():
                if v and kn.get(id(s), 0) < v:
                    self.E[e].wait_ge(s, v)
                    kn[id(s)] = v
        self.bufs = {}


def build_nc(S, L, dbg=False):
    NT = S // 128
    NTB = S // 512
    NCH = S // 32
    NB = NT + 32
    NR = NB * 512
    nc = bass.Bass("TRN2", target_bir_lowering=False)

    def din(name, shape, dt=F32):
        return nc.dram_tensor(name, shape, dt, kind="ExternalInput").ap()

    x_in = din("x", [S, D])
    w_in = din("w_in", [L * D, 3584])
    smallp = din("smallp", [60 * L, 128])
    lru_wa = din("lru_wa", [L * 2 * 8 * 64, 64])
    lru_wx = din("lru_wx", [L * 2 * 8 * 64, 64])
    w_out = din("w_out", [L * D, D])
    lnp = din("lnp", [L * 4, D])
    router_w = din("router_w", [L * D, NE])
    router_b = din("router_b", [L, NE])
    w_gu = din("w_gate_up", [L * NE * D, 2 * D])
    b_gu = din("b_gate_up", [L * NE, 2 * D])
    w_dn = din("w_down", [L * NE * D, D])
    b_dn = din("b_down", [L * NE, D])
    consts = din("consts", [128, NCON])
    out = nc.dram_tensor("out", [S, D], F32, kind="ExternalOutput").ap()

    def dscr(name, shape, dt):
        kind = "ExternalOutput" if dbg else "Internal"
        return nc.dram_tensor(name, shape, dt, kind=kind).ap()

    xT_d = dscr("xT_d", [8, 128, S], BF16)
    mixT_d = dscr("mixT_d", [8, 128, S], BF16)
    xres_d = dscr("xres_d", [S, D], F32)
    x1_d = dscr("x1_d", [S, D], F32)
    x1b_d = dscr("x1b_d", [S, D], BF16)
    xg_d = dscr("xg_d", [NR, D], BF16)
    yg_d = dscr("yg_d", [NR, D], F32)

    es = ExitStack()
    P = Prog(nc, es)
    dumped = set()

    def dump(name, ap, shape, dt, keys):
        if not dbg or name in dumped:
            return
        dumped.add(name)
        d = nc.dram_tensor("D_" + name, shape, dt, kind="ExternalOutput").ap()
        P.dma('sp', lambda e: e.dma_start(out=d, in_=ap), keys, [('dump', name)], 'dump_' + name)

    uid = [0]

    def sb(name, shape, dt, st=es):
        uid[0] += 1
        return st.enter_context(nc.sbuf_tensor("%s_%d" % (name, uid[0]), shape, dt))

    def ps(name, shape, dt, st=es):
        uid[0] += 1
        return st.enter_context(nc.psum_tensor("%s_%d" % (name, uid[0]), shape, dt))

    cst = sb("cst", [128, NCON], F32)
    ident = cst[:, 0:128]
    tri = [cst[0:32, 128:256], cst[0:32, 256:384]]
    iota_p = cst[:, 384:385]
    thr = cst[:, 385:385 + 64]
    iota_e = cst[:, 449:449 + 32]
    identb = sb("identb", [128, 128], BF16)
    ones_f = sb("ones_f", [128, 128], F32)
    ones_b = sb("ones_b", [128, 512], BF16)
    cmask = sb("cmask", [128, S], BF16)
    SP = sb("SP", [128, 60 * L], F32)
    LOW = sb("LOW", [128, L * 8], F32)
    OML = sb("OML", [128, L * 8], F32)
    CP = sb("CP", [128, L * 8], F32)
    CP2 = sb("CP2", [128, L * 8], F32)
    ssq_all = sb("ssq_all", [128, 8, NT], F32)
    rs_all = sb("rs_all", [128, 5, NT], F32)

    P.dma('sp', lambda e: e.dma_start(out=cst[:], in_=consts), [], ['cst'], 'cst')
    P.op('dve', lambda e: e.tensor_copy(out=identb[:], in_=ident), ['cst'], ['identb'])
    P.op('dve', lambda e: e.memset(ones_f[:], 1.0), [], ['ones_f'])
    P.op('dve', lambda e: e.memset(ones_b[:], 1.0), [], ['ones_b'])
    P.op('dve', lambda e: e.memset(cmask[:], 1.0), [], ['cmask'])
    P.op('dve', lambda e: e.memset(cmask[:].rearrange("p (c j) -> p c j", j=32)[:, :, 0:1], 0.0), [], ['cmask'])

    off_lb, off_hn, off_cw, off_cb = 0, L * 8, L * 12, L * 28
    off_ba, off_bx, off_lam, off_ln = L * 32, L * 40, L * 48, L * 56
    with ExitStack() as ph:
        stg = sb("stg", [128, 128], F32, ph)
        pst = ps("pst", [128, 512], F32, ph)
        R = 60 * L
        r0 = 0
        while r0 < R:
            n = min(128, R - r0)
            P.dma('sp', lambda e, r0=r0, n=n: e.dma_start(out=stg[0:n, :], in_=smallp[r0:r0 + n, :]), [], ['stg'], 'stg')
            P.op('pe', lambda e, n=n: e.transpose(out=pst[:, 0:n], in_=stg[0:n, :], identity=ident[0:n, 0:n]), ['stg', 'cst'], ['pst'])
            P.op('dve', lambda e, r0=r0, n=n: e.tensor_copy(out=SP[:, r0:r0 + n], in_=pst[:, 0:n]), ['pst'], ['SP'])
            r0 += n
        tmp = sb("lbtmp", [128, L * 8], F32, ph)
        mx = sb("lbmx", [128, 8], F32, ph)
        sm = sb("lbsm", [128, 8], F32, ph)
        lb = SP[:, off_lb:off_lb + L * 8]
        P.op('dve', lambda e: e.tensor_copy(out=mx[:], in_=lb[:, 0:8]), ['SP'], ['mx'])
        for l in range(1, L):
            P.op('dve', lambda e, l=l: e.tensor_max(out=mx[:], in0=mx[:], in1=lb[:, l * 8:(l + 1) * 8]), ['SP', 'mx'], ['mx'])
        for l in range(L):
            P.op('dve', lambda e, l=l: e.tensor_sub(out=tmp[:, l * 8:(l + 1) * 8], in0=lb[:, l * 8:(l + 1) * 8], in1=mx[:]), ['SP', 'mx'], ['lbtmp'])
        P.op('act', lambda e: e.activation(out=tmp[:], in_=tmp[:], func=AF.Exp), ['lbtmp'], ['lbtmp'])
        P.op('dve', lambda e: e.tensor_copy(out=sm[:], in_=tmp[:, 0:8]), ['lbtmp'], ['sm'])
        for l in range(1, L):
            P.op('dve', lambda e, l=l: e.tensor_add(out=sm[:], in0=sm[:], in1=tmp[:, l * 8:(l + 1) * 8]), ['lbtmp', 'sm'], ['sm'])
        P.op('dve', lambda e: e.reciprocal(out=sm[:], in_=sm[:]), ['sm'], ['sm'])
        P.op('dve', lambda e: e.memset(LOW[:], 0.0), [], ['LOW'])
        for l in range(1, L):
            P.op('dve', lambda e, l=l: e.tensor_mul(out=tmp[:, l * 8:(l + 1) * 8], in0=tmp[:, l * 8:(l + 1) * 8], in1=sm[:]), ['lbtmp', 'sm'], ['lbtmp'])
            P.op('dve', lambda e, l=l: e.tensor_add(out=LOW[:, l * 8:(l + 1) * 8], in0=LOW[:, (l - 1) * 8:l * 8], in1=tmp[:, l * 8:(l + 1) * 8]), ['lbtmp', 'LOW'], ['LOW'])
        P.op('dve', lambda e: e.tensor_scalar(out=LOW[:], in0=LOW[:], scalar1=0.0, scalar2=1.0 - 1e-6, op0=ALU.max, op1=ALU.min), ['LOW'], ['LOW'])
        P.op('dve', lambda e: e.tensor_scalar(out=OML[:], in0=LOW[:], scalar1=-1.0, scalar2=1.0, op0=ALU.mult, op1=ALU.add), ['LOW'], ['OML'])
        lam = SP[:, off_lam:off_lam + L * 8]
        P.op('act', lambda e: e.activation(out=CP[:], in_=lam, func=AF.Sigmoid), ['SP'], ['CP'])
        P.op('act', lambda e: e.activation(out=CP[:], in_=CP[:], func=AF.Ln), ['CP'], ['CP'])
        P.op('dve', lambda e: e.tensor_scalar(out=CP2[:], in0=CP[:], scalar1=16.0, scalar2=None, op0=ALU.mult), ['CP'], ['CP2'])
        P.op('dve', lambda e: e.tensor_scalar(out=CP[:], in0=CP[:], scalar1=8.0, scalar2=None, op0=ALU.mult), ['CP', 'CP2'], ['CP'])
        dump("SP", SP[:], [128, 60 * L], F32, ['SP'])
        dump("LOW", LOW[:], [128, L * 8], F32, ['LOW'])
        dump("CP", CP[:], [128, L * 8], F32, ['CP'])
        P.barrier()

    def emit_xT(st, src, srckey, i, pT, xTb, par):
        for k in range(8):
            P.op('pe', lambda e, k=k: e.transpose(out=pT[:, k * 128:(k + 1) * 128], in_=src[:, k * 128:(k + 1) * 128], identity=ident),
                 [srckey, 'cst'], [('pT', par)])
        P.op('act', lambda e: e.activation(out=xTb[:].rearrange("p k t -> p (k t)"), in_=pT[:, :], func=AF.Copy), [('pT', par)], [('xTb', par)])
        P.dma('sp', lambda e: e.dma_start(out=xT_d[:, :, i * 128:(i + 1) * 128].rearrange("k p t -> p k t"), in_=xTb[:]),
              [('xTb', par)], [('xT_d', i // 4)], 'xTb%d' % par)

    with ExitStack() as ph:
        xt = [sb("p0x%d" % j, [128, D], F32, ph) for j in range(2)]
        xTb = [sb("p0b%d" % j, [128, 8, 128], BF16, ph) for j in range(2)]
        pT = [ps("p0p%d" % j, [128, 1024], F32, ph) for j in range(2)]
        for i in range(NT):
            j = i % 2
            P.dma('sp', lambda e, i=i, j=j: e.dma_start(out=xt[j][:], in_=x_in[i * 128:(i + 1) * 128, :]), [], [('xt', j)], 'p0x%d' % j)
            emit_xT(ph, xt[j], ('xt', j), i, pT[j], xTb[j], j)
        P.barrier()

    for l in range(L):
        xcur = x_in if l == 0 else xres_d
        xnext = out if l == L - 1 else xres_d

        for h in range(4):
            with ExitStack() as ph:
                q = sb("hq", [128, S], BF16, ph)
                v = sb("hv", [128, S], BF16, ph)
                sg = sb("hsg", [128, S], BF16, ph)
                logf = sb("hlogf", [128, S], F32, ph)
                kk = sb("hkk", [128, S], BF16, ph)
                g = sb("hg", [128, S], F32, ph)
                Eb = sb("hE", [128, S], F32, ph)
                A2 = [sb("hA%d" % d, [128, S], BF16, ph) for d in range(2)]
                Bk2 = [sb("hBk%d" % d, [128, S], BF16, ph) for d in range(2)]
                Kh2 = [sb("hKh%d" % d, [128, S], BF16, ph) for d in range(2)]
                oT = sb("hoT", [128, S], F32, ph)
                wq = sb("hwq", [128, 8, 5 * 128], BF16, ph)
                xb = [sb("hxb%d" % j, [128, 8, 512], BF16, ph) for j in range(2)]
                t1 = [sb("ht1%d" % j, [128, 512], F32, ph) for j in range(2)]
                eref2 = [sb("heref%d" % d, [128, NCH], F32, ph) for d in range(2)]
                dec2 = [sb("hdec%d" % d, [128, NCH], F32, ph) for d in range(2)]
                c22 = [sb("hc2%d" % d, [128, NCH], F32, ph) for d in range(2)]
                Sf2 = [sb("hSf%d" % d, [128, 128], F32, ph) for d in range(2)]
                Sb2 = [sb("hSb%d" % d, [128, 128], BF16, ph) for d in range(2)]
                scm2 = [[sb("hscm%d%d" % (d, j), [32, 128], BF16, ph) for j in range(2)] for d in range(2)]
                TT2 = [[sb("hTT%d%d" % (d, j), [32, 1024], BF16, ph) for j in range(2)] for d in range(2)]
                pz = [ps("hpz%d" % j, [128, 512], F32, ph) for j in range(2)]
                ps_sc1 = ps("hpsc", [128, 512], F32, ph)
                ps_t1 = ps("hpt", [128, 1024], BF16, ph)
                psk = [ps("hpk%d" % d, [128, 512], F32, ph) for d in range(2)]
                kcount = [0, 0]
                ps_o2 = [ps("hpo%d" % d, [128, 512], F32, ph) for d in range(2)]

                cols = [h * 128, 512 + h * 128, 1024 + h * 128, 1536 + h * 128, 2048 + h * 128]
                for z in range(5):
                    P.dma('pool', lambda e, z=z: e.dma_start(
                        out=wq[:, :, z * 128:(z + 1) * 128],
                        in_=w_in[l * D:(l + 1) * D, cols[z]:cols[z] + 128].rearrange("(k p) c -> p k c", p=128)),
                        [], ['wq'], 'wq')

                def project(zs):
                    for tb in range(NTB):
                        j = tb % 2
                        tsl = slice(tb * 512, (tb + 1) * 512)
                        P.dma('sp', lambda e, tb=tb, j=j: e.dma_start(out=xb[j][:], in_=xT_d[:, :, tb * 512:(tb + 1) * 512].rearrange("k p t -> p k t")),
                              [('xT_d', tb)], [('xb', j)], 'hxb%d' % j)
                        for zi, z in enumerate(zs):
                            pj = (tb * len(zs) + zi) % 2
                            for k in range(8):
                                P.op('pe', lambda e, k=k, z=z, j=j, pj=pj: e.matmul(out=pz[pj][:, :], lhsT=wq[:, k, z * 128:(z + 1) * 128], rhs=xb[j][:, k, :], start=(k == 0), stop=(k == 7)),
                                     ['wq', ('xb', j)], [('pz', pj)])
                            if z == 0:
                                P.op('act', lambda e, pj=pj, tsl=tsl: e.activation(out=q[:, tsl], in_=pz[pj][:, :], func=AF.Silu), [('pz', pj)], ['q'])
                            elif z == 1:
                                P.op('act', lambda e, pj=pj, tsl=tsl: e.activation(out=v[:, tsl], in_=pz[pj][:, :], func=AF.Copy), [('pz', pj)], ['v'])
                            elif z == 4:
                                P.op('act', lambda e, pj=pj, tsl=tsl: e.activation(out=sg[:, tsl], in_=pz[pj][:, :], func=AF.Silu), [('pz', pj)], ['sg'])
                            else:
                                P.op('act', lambda e, pj=pj, tsl=tsl: e.activation(out=logf[:, tsl], in_=pz[pj][:, :], func=AF.Sigmoid), [('pz', pj)], ['logf'])

                def gate_bulk(r):
                    col = l * 8 + r * 4 + h
                    P.op('dve', lambda e: e.tensor_scalar(out=logf[:], in0=logf[:], scalar1=OML[:, col:col + 1], scalar2=LOW[:, col:col + 1], op0=ALU.mult, op1=ALU.add),
                         ['logf', 'OML', 'LOW'], ['logf'])
                    P.op('dve', lambda e: e.tensor_scalar(out=kk[:], in0=logf[:], scalar1=-1.0, scalar2=1.0, op0=ALU.mult, op1=ALU.add), ['logf'], ['kk'])
                    P.op('act', lambda e: e.activation(out=logf[:], in_=logf[:], func=AF.Ln), ['logf', 'kk'], ['logf'])

                def prep(dr):
                    fwd = dr == 0
                    A, Bk, eref, dec, c2 = A2[dr], Bk2[dr], eref2[dr], dec2[dr], c22[dr]
                    V = (lambda ap: ap) if fwd else (lambda ap: ap[:, ::-1])
                    P.op('dve', lambda e: e.tensor_tensor_scan(out=V(g[:]), data0=cmask[:], data1=V(logf[:]), initial=0.0, op0=ALU.mult, op1=ALU.add),
                         ['cmask', 'logf'], ['g'])
                    g3 = g[:].rearrange("p (c j) -> p c j", j=32)
                    jm, jl = (15, 31) if fwd else (16, 0)
                    gref = g3[:, :, jm]
                    glast = g3[:, :, jl]
                    P.op('act', lambda e: e.activation(out=eref[:], in_=gref, func=AF.Exp), ['g'], [('eref', dr)])
                    P.op('act', lambda e: e.activation(out=dec[:], in_=glast, func=AF.Exp), ['g'], [('dec', dr)])
                    E3 = Eb[:].rearrange("p (c j) -> p c j", j=32)
                    P.op('dve', lambda e: e.tensor_sub(out=E3, in0=g3[:, :, jl:jl + 1].to_broadcast([128, NCH, 32]), in1=g3), ['g'], ['E'])
                    P.op('act', lambda e: e.activation(out=Eb[:], in_=Eb[:], func=AF.Exp), ['E'], ['E'])
                    P.op('dve', lambda e: e.tensor_mul(out=Kh2[dr][:], in0=kk[:], in1=Eb[:]), ['kk', 'E'], [('Kh', dr)])
                    P.op('dve', lambda e: e.tensor_sub(out=E3, in0=g3, in1=g3[:, :, jm:jm + 1].to_broadcast([128, NCH, 32])), ['g', 'E'], ['E'])
                    P.op('act', lambda e: e.activation(out=g[:], in_=Eb[:], func=AF.Exp), ['E', 'g', ('eref', dr), ('dec', dr)], ['g'])
                    P.op('act', lambda e: e.activation(out=Eb[:], in_=Eb[:], func=AF.Exp, scale=-1.0), ['E', 'g'], ['E'])
                    P.op('dve', lambda e: e.tensor_mul(out=A[:], in0=q[:], in1=g[:]), ['q', 'g'], [('A', dr)])
                    P.op('dve', lambda e: e.tensor_mul(out=Bk[:], in0=kk[:], in1=Eb[:]), ['kk', 'E'], [('Bk', dr)])
                    P.op('dve', lambda e: e.memset(Sf2[dr][:], 0.0), [], [('Sf', dr)])
                    P.op('dve', lambda e: e.memset(Sb2[dr][:], 0.0), [], [('Sb', dr)])

                def tile_head(dr, n_i, i):
                    A, Bk = A2[dr], Bk2[dr]
                    j = n_i % 2
                    scm, TT, ps_sc, ps_t = scm2[dr][j], TT2[dr][j], ps_sc1, ps_t1
                    for cc in range(4):
                        csl = slice((4 * i + cc) * 32, (4 * i + cc + 1) * 32)
                        P.op('pe', lambda e, cc=cc, csl=csl: e.matmul(out=ps_sc[0:32, cc * 32:(cc + 1) * 32], lhsT=Bk[:, csl], rhs=A[:, csl], start=True, stop=True),
                             [('Bk', dr), ('A', dr)], ['ps_sc'])
                    P.op('dve', lambda e: e.tensor_mul(out=scm[:], in0=ps_sc[0:32, 0:128], in1=tri[dr]), ['ps_sc', 'cst'], [('scm', dr, j)])
                    for cc in range(4):
                        csl = slice((4 * i + cc) * 32, (4 * i + cc + 1) * 32)
                        P.op('pe', lambda e, cc=cc, csl=csl: e.transpose(out=ps_t[0:32, cc * 128:(cc + 1) * 128], in_=Kh2[dr][:, csl], identity=identb[:]),
                             [('Kh', dr), 'identb'], ['ps_t'])
                    for cc in range(4):
                        csl = slice((4 * i + cc) * 32, (4 * i + cc + 1) * 32)
                        P.op('pe', lambda e, cc=cc, csl=csl: e.transpose(out=ps_t[0:32, 512 + cc * 128:512 + (cc + 1) * 128], in_=v[:, csl], identity=identb[:]),
                             ['v', 'identb'], ['ps_t'])
                    P.op('dve', lambda e: e.tensor_copy(out=TT[:], in_=ps_t[0:32, 0:1024]), ['ps_t'], [('TT', dr, j)])

                def emit_kv(dr, n_i, cc):
                    j = n_i % 2
                    TT = TT2[dr][j]
                    par = kcount[dr] % 2
                    kcount[dr] += 1
                    buf, key = (pz[dr], ('pz', dr)) if par == 0 else (psk[dr], ('psk', dr))
                    P.op('pe', lambda e: e.matmul(out=buf[:, 0:128], lhsT=TT[:, cc * 128:(cc + 1) * 128], rhs=TT[:, 512 + cc * 128:512 + (cc + 1) * 128], start=True, stop=True),
                         [('TT', dr, j)], [key])
                    return buf, key

                def chunk(dr, n_i, i, cc, kvb):
                    fwd = dr == 0
                    A = A2[dr]
                    eref, dec, Sf, Sb = eref2[dr], dec2[dr], Sf2[dr], Sb2[dr]
                    j = n_i % 2
                    scm, TT, ps_o = scm2[dr][j], TT2[dr][j], ps_o2[dr]
                    ps_kv, kvkey = kvb
                    vT = TT[:, 512:1024]
                    ch = 4 * i + cc
                    csl = slice(ch * 32, (ch + 1) * 32)
                    P.op('pe', lambda e: e.matmul(out=ps_o[:, cc * 32:(cc + 1) * 32], lhsT=vT[:, cc * 128:(cc + 1) * 128], rhs=scm[:, cc * 32:(cc + 1) * 32], start=True, stop=False),
                         [('TT', dr, j), ('scm', dr, j)], [('ps_o', dr)])
                    P.op('pe', lambda e: e.matmul(out=ps_o[:, cc * 32:(cc + 1) * 32], lhsT=Sb[:], rhs=A[:, csl], start=False, stop=True),
                         [('Sb', dr), ('A', dr)], [('ps_o', dr)])
                    P.op('dve', lambda e: e.scalar_tensor_tensor(out=Sf[:], in0=Sf[:], scalar=dec[:, ch:ch + 1], in1=ps_kv[:, 0:128], op0=ALU.mult, op1=ALU.add),
                         [kvkey, ('dec', dr), ('Sf', dr)], [('Sf', dr)])
                    chn = ch + 1 if fwd else ch - 1
                    if 0 <= chn < NCH:
                        P.op('act', lambda e: e.activation(out=Sb[:], in_=Sf[:], func=AF.Copy, scale=eref[:, chn:chn + 1]), [('Sf', dr), ('eref', dr)], [('Sb', dr)])

                def tile_tail(dr, i):
                    tsl = slice(i * 128, (i + 1) * 128)
                    P.op('dve', lambda e: e.tensor_add(out=oT[:, tsl], in0=oT[:, tsl], in1=ps_o2[dr][:, 0:128]), [('ps_o', dr), ('oT', i)], [('oT', i)])

                project([0, 4])
                project([1, 2])
                gate_bulk(0)
                prep(0)
                project([3])
                gate_bulk(1)
                prep(1)
                P.op('dve', lambda e: e.memset(oT[:], 0.0), [], [('oT', i) for i in range(NT)])
                for n_i in range(NT):
                    tiles = (n_i, NT - 1 - n_i)
                    for dr in range(2):
                        tile_head(dr, n_i, tiles[dr])
                    order = ((0, 1, 2, 3), (3, 2, 1, 0))
                    kvb = [emit_kv(dr, n_i, order[dr][0]) for dr in range(2)]
                    for step in range(4):
                        for dr in range(2):
                            nxt = emit_kv(dr, n_i, order[dr][step + 1]) if step < 3 else None
                            chunk(dr, n_i, tiles[dr], order[dr][step], kvb[dr])
                            kvb[dr] = nxt
                    for dr in range(2):
                        tile_tail(dr, tiles[dr])
                okeys = [('oT', i) for i in range(NT)]
                P.op('act', lambda e: e.activation(out=Eb[:], in_=oT[:], func=AF.Square), okeys, ['E'])
                for i in range(NT):
                    P.op('pe', lambda e, i=i: e.matmul(out=ps_sc1[:, 256 + i:257 + i], lhsT=Eb[:, i * 128:(i + 1) * 128], rhs=ones_f[:, 0:1], start=True, stop=True),
                         ['E', 'ones_f'], ['ps_sc'])
                P.op('dve', lambda e: e.tensor_copy(out=ssq_all[:, h, :], in_=ps_sc1[:, 256:256 + NT]), ['ps_sc'], ['ssq_all'])
                gn = SP[:, off_hn + l * 4 + h:off_hn + l * 4 + h + 1]
                P.op('dve', lambda e: e.scalar_tensor_tensor(out=A2[0][:], in0=oT[:], scalar=gn, in1=sg[:], op0=ALU.mult, op1=ALU.mult), okeys + ['sg', 'SP'], [('A', 0)])
                P.dma('sp', lambda e: e.dma_start(out=mixT_d[h, :, :], in_=A2[0][:]), [('A', 0)], [('mixT_d', h)], 'hA')
                P.barrier()

        for c in range(4):
            with ExitStack() as ph:
                zx = sb("lzx", [128, S + 4], F32, ph)
                xc = sb("lxc", [128, S], F32, ph)
                xcb = sb("lxcb", [128, S], BF16, ph)
                gy = sb("lgy", [128, S], BF16, ph)
                ua = sb("lua", [128, S], F32, ph)
                uu = sb("luu", [128, S], F32, ph)
                hf = sb("lhf", [128, S], F32, ph)
                hb = sb("lhb", [128, S], F32, ph)
                wz = sb("lwz", [128, 8, 256], BF16, ph)
                wbd = sb("lwbd", [128, 4, 128], BF16, ph)
                xb = [sb("lxb%d" % j, [128, 8, 512], BF16, ph) for j in range(2)]
                t1 = [sb("lt1%d" % j, [128, 512], F32, ph) for j in range(2)]
                t2 = [sb("lt2%d" % j, [128, 512], F32, ph) for j in range(2)]
                pz = [ps("lpz%d" % j, [128, 512], F32, ph) for j in range(4)]
                ps_ss = ps("lpss", [128, 512], F32, ph)

                for z in range(2):
                    colz = 2560 + z * 512 + c * 128
                    P.dma('pool', lambda e, z=z, colz=colz: e.dma_start(
                        out=wz[:, :, z * 128:(z + 1) * 128],
                        in_=w_in[l * D:(l + 1) * D, colz:colz + 128].rearrange("(k p) c -> p k c", p=128)), [], ['wz'], 'lwz')
                P.op('dve', lambda e: e.memset(wbd[:], 0.0), [], ['wbd'])
                for r in range(2):
                    for wi, wsrc in enumerate((lru_wa, lru_wx)):
                        for hh in range(2):
                            row0 = ((l * 2 + r) * 8 + 2 * c + hh) * 64
                            P.dma('pool', lambda e, r=r, wi=wi, hh=hh, row0=row0, wsrc=wsrc: e.dma_start(
                                out=wbd[hh * 64:(hh + 1) * 64, r * 2 + wi, hh * 64:(hh + 1) * 64], in_=wsrc[row0:row0 + 64, :]), [], ['wbd'], 'lwbd')
                P.op('dve', lambda e: e.memset(zx[:, 0:2], 0.0), [], ['zx'])
                P.op('dve', lambda e: e.memset(zx[:, S + 2:S + 4], 0.0), [], ['zx'])
                for tb in range(NTB):
                    j = tb % 2
                    tsl = slice(tb * 512, (tb + 1) * 512)
                    P.dma('sp', lambda e, tb=tb, j=j: e.dma_start(out=xb[j][:], in_=xT_d[:, :, tb * 512:(tb + 1) * 512].rearrange("k p t -> p k t")),
                          [('xT_d', tb)], [('xb', j)], 'lxb%d' % j)
                    for z in range(2):
                        pj = z + 2 * j
                        for k in range(8):
                            P.op('pe', lambda e, k=k, z=z, j=j, pj=pj: e.matmul(out=pz[pj][:, :], lhsT=wz[:, k, z * 128:(z + 1) * 128], rhs=xb[j][:, k, :], start=(k == 0), stop=(k == 7)),
                                 ['wz', ('xb', j)], [('pz', pj)])
                    P.op('act', lambda e, j=j, tb=tb: e.activation(out=zx[:, 2 + tb * 512:2 + (tb + 1) * 512], in_=pz[2 * j][:, :], func=AF.Copy), [('pz', 2 * j)], ['zx'])
                    pj = 1 + 2 * j
                    P.op('act', lambda e, j=j, pj=pj: e.activation(out=t1[j][:], in_=pz[pj][:, :], func=AF.Square), [('pz', pj)], [('t1', j)])
                    P.op('dve', lambda e, j=j: e.tensor_scalar(out=t1[j][:], in0=t1[j][:], scalar1=0.044715 * 1.5957691216, scalar2=1.5957691216, op0=ALU.mult, op1=ALU.add), [('t1', j)], [('t1', j)])
                    P.op('dve', lambda e, j=j, pj=pj: e.tensor_mul(out=t1[j][:], in0=t1[j][:], in1=pz[pj][:, :]), [('t1', j), ('pz', pj)], [('t1', j)])
                    P.op('act', lambda e, j=j: e.activation(out=t1[j][:], in_=t1[j][:], func=AF.Sigmoid), [('t1', j)], [('t1', j)])
                    P.op('dve', lambda e, j=j, pj=pj, tsl=tsl: e.tensor_mul(out=gy[:, tsl], in0=t1[j][:], in1=pz[pj][:, :]), [('t1', j), ('pz', pj)], ['gy'])
                cw = lambda jj: SP[:, off_cw + (l * 4 + jj) * 4 + c:off_cw + (l * 4 + jj) * 4 + c + 1]
                cb = SP[:, off_cb + l * 4 + c:off_cb + l * 4 + c + 1]
                P.op('dve', lambda e: e.tensor_scalar(out=xc[:], in0=zx[:, 0:S], scalar1=cw(0), scalar2=cb, op0=ALU.mult, op1=ALU.add), ['zx', 'SP'], ['xc'])
                for jj in range(1, 4):
                    P.op('dve', lambda e, jj=jj: e.scalar_tensor_tensor(out=xc[:], in0=zx[:, jj:jj + S], scalar=cw(jj), in1=xc[:], op0=ALU.mult, op1=ALU.add), ['zx', 'SP', 'xc'], ['xc'])
                P.op('act', lambda e: e.activation(out=xcb[:], in_=xc[:], func=AF.Copy), ['xc'], ['xcb'])
                for r in range(2):
                    col = l * 8 + r * 4 + c
                    ba = SP[:, off_ba + col:off_ba + col + 1]
                    bx = SP[:, off_bx + col:off_bx + col + 1]
                    hdst = hf if r == 0 else hb
                    hkey = 'hf' if r == 0 else 'hb'
                    for tb in range(NTB):
                        j = tb % 2
                        tsl = slice(tb * 512, (tb + 1) * 512)
                        P.op('pe', lambda e, j=j, tsl=tsl: e.matmul(out=pz[2 * j][:, :], lhsT=wbd[:, r * 2, :], rhs=xcb[:, tsl], start=True, stop=True), ['wbd', 'xcb'], [('pz', 2 * j)])
                        P.op('pe', lambda e, j=j, tsl=tsl: e.matmul(out=pz[2 * j + 1][:, :], lhsT=wbd[:, r * 2 + 1, :], rhs=xcb[:, tsl], start=True, stop=True), ['wbd', 'xcb'], [('pz', 2 * j + 1)])
                        P.op('act', lambda e, j=j, tsl=tsl: e.activation(out=ua[:, tsl], in_=pz[2 * j][:, :], func=AF.Sigmoid, bias=ba), [('pz', 2 * j), 'SP'], ['ua'])
                        P.op('act', lambda e, j=j, tsl=tsl: e.activation(out=uu[:, tsl], in_=pz[2 * j + 1][:, :], func=AF.Sigmoid, bias=bx), [('pz', 2 * j + 1), 'SP'], ['uu'])
                    P.op('act', lambda e: e.activation(out=hdst[:], in_=ua[:], func=AF.Exp, scale=CP2[:, col:col + 1]), ['ua', 'CP2'], [hkey])
                    P.op('act', lambda e: e.activation(out=ua[:], in_=ua[:], func=AF.Exp, scale=CP[:, col:col + 1]), ['ua', 'CP', hkey], ['ua'])
                    P.op('dve', lambda e: e.tensor_scalar(out=hdst[:], in0=hdst[:], scalar1=-1.0, scalar2=1.0, op0=ALU.mult, op1=ALU.add), [hkey], [hkey])
                    P.op('dve', lambda e: e.tensor_scalar(out=hdst[:], in0=hdst[:], scalar1=0.0, scalar2=None, op0=ALU.max), [hkey], [hkey])
                    P.op('act', lambda e: e.activation(out=hdst[:], in_=hdst[:], func=AF.Sqrt), [hkey], [hkey])
                    P.op('dve', lambda e: e.tensor_mul(out=uu[:], in0=uu[:], in1=xc[:]), ['uu', 'xc'], ['uu'])
                    P.op('dve', lambda e: e.tensor_mul(out=uu[:], in0=uu[:], in1=hdst[:]), ['uu', hkey], ['uu'])
                    V = (lambda ap: ap) if r == 0 else (lambda ap: ap[:, ::-1])
                    P.op('dve', lambda e, V=V, hdst=hdst: e.tensor_tensor_scan(out=V(hdst[:]), data0=V(ua[:]), data1=V(uu[:]), initial=0.0, op0=ALU.mult, op1=ALU.add),
                         ['ua', 'uu', hkey], [hkey])
                dump("zx", zx[:], [128, S + 4], F32, ['zx'])
                dump("xc", xc[:], [128, S], F32, ['xc'])
                dump("ua", ua[:], [128, S], F32, ['ua'])
                dump("uu", uu[:], [128, S], F32, ['uu'])
                dump("hf", hf[:], [128, S], F32, ['hf'])
                dump("hb", hb[:], [128, S], F32, ['hb'])
                dump("gy", gy[:], [128, S], BF16, ['gy'])
                P.op('dve', lambda e: e.tensor_add(out=hf[:], in0=hf[:], in1=hb[:]), ['hf', 'hb'], ['hf'])
                P.op('act', lambda e: e.activation(out=hb[:], in_=hf[:], func=AF.Square), ['hf', 'hb'], ['hb'])
                for i in range(NT):
                    P.op('pe', lambda e, i=i: e.matmul(out=ps_ss[:, i:i + 1], lhsT=hb[:, i * 128:(i + 1) * 128], rhs=ones_f[:, 0:1], start=True, stop=True), ['hb', 'ones_f'], ['ps_ss'])
                P.op('dve', lambda e: e.tensor_copy(out=ssq_all[:, 4 + c, :], in_=ps_ss[:, 0:NT]), ['ps_ss'], ['ssq_all'])
                gn = SP[:, off_ln + l * 4 + c:off_ln + l * 4 + c + 1]
                P.op('dve', lambda e: e.scalar_tensor_tensor(out=xcb[:], in0=hf[:], scalar=gn, in1=gy[:], op0=ALU.mult, op1=ALU.mult), ['hf', 'gy', 'SP'], ['xcb'])
                dump("xcbf", xcb[:], [128, S], BF16, ['xcb'])
                dump("hff", hf[:], [128, S], F32, ['hf'])
                P.dma('sp', lambda e: e.dma_start(out=mixT_d[4 + c, :, :], in_=xcb[:]), ['xcb'], [('mixT_d', 4 + c)], 'lxcb')
                P.barrier()

        with ExitStack() as pm:
            lg_all = sb("wlg", [128, NT, NE], F32, pm)
            bidx16 = sb("rbidx16", [128, NB], I32, pm)
            wk = sb("rwk", [128, NT, 4], F32, pm)
            desti = sb("rdesti", [128, NT, 4], I32, pm)
            widx = sb("rwidx", [128, 3, NB, 8], I32, pm)
            bidx = sb("rbidx", [128, NB], I32, pm)
            st6 = sb("wst6", [128, 2, 6], F32, pm)
            mv = sb("wmv", [128, 2], F32, pm)
            rstd = sb("wrstd", [128, 1], F32, pm)
            def load_lnr(st, r0):
                t = sb("wlnr", [128, 2, D], F32, st)
                for r4 in range(2):
                    P.dma('sp', lambda e, r4=r4: e.dma_start(out=t[:, r4, :], in_=lnp[l * 4 + r0 + r4:l * 4 + r0 + r4 + 1, :].to_broadcast([128, D])), [], ['lnr'], 'wlnr')
                return t

            def layernorm(z, zkey, lnr, dst, dstkey):
                gi = 0
                zz = z[:].rearrange("p (a f) -> p a f", a=2)
                for a in range(2):
                    P.op('dve', lambda e, a=a: e.bn_stats(out=st6[:, a, :], in_=zz[:, a, :]), [zkey], ['st6'])
                P.op('dve', lambda e: e.bn_aggr(out=mv[:], in_=st6[:]), ['st6'], ['mv'])
                P.op('dve', lambda e: e.tensor_scalar(out=rstd[:], in0=mv[:, 1:2], scalar1=LN_EPS, scalar2=None, op0=ALU.add), ['mv'], ['rstd'])
                P.op('act', lambda e: e.activation(out=rstd[:], in_=rstd[:], func=AF.Sqrt), ['rstd'], ['rstd'])
                P.op('dve', lambda e: e.reciprocal(out=rstd[:], in_=rstd[:]), ['rstd'], ['rstd'])
                P.op('dve', lambda e: e.tensor_scalar(out=z[:], in0=z[:], scalar1=mv[:, 0:1], scalar2=rstd[:, 0:1], op0=ALU.subtract, op1=ALU.mult), [zkey, 'mv', 'rstd'], [zkey])
                P.op('dve', lambda e: e.tensor_mul(out=z[:], in0=z[:], in1=lnr[:, gi, :]), [zkey, 'lnr'], [zkey])
                P.op('dve', lambda e: e.tensor_add(out=dst[:], in0=z[:], in1=lnr[:, gi + 1, :]), [zkey, 'lnr'], [dstkey])

            with ExitStack() as ph:
                lnr1 = load_lnr(ph, 0)
                wo = sb("wwo", [128, 8, D], BF16, ph)
                rw = sb("wrw", [128, 8, NE], F32, ph)
                rb = sb("wrb", [128, NE], F32, ph)
                mt = [sb("wmt%d" % j, [128, 8, 128], BF16, ph) for j in range(2)]
                xt = [sb("wxt%d" % j, [128, D], F32, ph) for j in range(2)]
                acc = [sb("wacc%d" % j, [128, D], F32, ph) for j in range(2)]
                x1 = [sb("wx1%d" % j, [128, D], F32, ph) for j in range(2)]
                x1b = [sb("wx1b%d" % j, [128, D], BF16, ph) for j in range(2)]
                x1T = [sb("wx1T%d" % j, [128, D], F32, ph) for j in range(2)]
                py = [ps("wpy%d" % j, [128, 512], F32, ph) for j in range(5)]
                pT = ps("wpT", [128, 1024], F32, ph)
                plg = ps("wplg", [128, 512], F32, ph)

                P.dma('pool', lambda e: e.dma_start(out=wo[:], in_=w_out[l * D:(l + 1) * D, :].rearrange("(k p) c -> p k c", p=128)), [], ['wo'], 'wwo')
                P.dma('sp', lambda e: e.dma_start(out=rw[:], in_=router_w[l * D:(l + 1) * D, :].rearrange("(k p) c -> p k c", p=128)), [], ['rw'], 'wrw')
                P.dma('sp', lambda e: e.dma_start(out=rb[:], in_=router_b[l:l + 1, :].to_broadcast([128, NE])), [], ['rb'], 'wrb')
                P.op('dve', lambda e: e.tensor_scalar(out=rs_all[:, 0:4, :], in0=ssq_all[:, 0:4, :], scalar1=1.0 / 128, scalar2=RMS_EPS, op0=ALU.mult, op1=ALU.add), ['ssq_all'], ['rs_all'])
                P.op('dve', lambda e: e.tensor_add(out=rs_all[:, 4, :], in0=ssq_all[:, 4, :], in1=ssq_all[:, 5, :]), ['ssq_all', 'rs_all'], ['rs_all'])
                P.op('dve', lambda e: e.tensor_add(out=rs_all[:, 4, :], in0=rs_all[:, 4, :], in1=ssq_all[:, 6, :]), ['ssq_all', 'rs_all'], ['rs_all'])
                P.op('dve', lambda e: e.tensor_add(out=rs_all[:, 4, :], in0=rs_all[:, 4, :], in1=ssq_all[:, 7, :]), ['ssq_all', 'rs_all'], ['rs_all'])
                P.op('dve', lambda e: e.tensor_scalar(out=rs_all[:, 4, :], in0=rs_all[:, 4, :], scalar1=1.0 / 512, scalar2=RMS_EPS, op0=ALU.mult, op1=ALU.add), ['rs_all'], ['rs_all'])
                P.op('act', lambda e: e.activation(out=rs_all[:], in_=rs_all[:], func=AF.Sqrt), ['rs_all'], ['rs_all'])
                P.op('dve', lambda e: e.reciprocal(out=rs_all[:], in_=rs_all[:]), ['rs_all'], ['rs_all'])

                for i in range(NT):
                    j = i % 2
                    rsl = slice(i * 128, (i + 1) * 128)
                    P.dma('sp', lambda e, i=i, j=j: e.dma_start(out=mt[j][:], in_=mixT_d[:, :, i * 128:(i + 1) * 128].rearrange("k p t -> p k t")),
                          [], [('mt', j)], 'wmt%d' % j)
                    P.dma('sp', lambda e, j=j, rsl=rsl: e.dma_start(out=xt[j][:], in_=xcur[rsl, :]), [], [('xt', j)], 'wxt%d' % j)
                    for dh in range(2):
                        dsl = slice(dh * 512, (dh + 1) * 512)
                        for gq in range(5):
                            ks = [gq] if gq < 4 else [4, 5, 6, 7]
                            for ki, k in enumerate(ks):
                                P.op('pe', lambda e, k=k, gq=gq, j=j, dsl=dsl, ki=ki, n=len(ks): e.matmul(out=py[gq][:, :], lhsT=mt[j][:, k, :], rhs=wo[:, k, dsl], start=(ki == 0), stop=(ki == n - 1)),
                                     ['wo', ('mt', j)], [('py', gq)])
                        P.op('dve', lambda e, j=j, dsl=dsl, i=i: e.tensor_scalar(out=acc[j][:, dsl], in0=py[0][:, :], scalar1=rs_all[:, 0, i:i + 1], scalar2=None, op0=ALU.mult), [('py', 0), 'rs_all'], [('acc', j)])
                        for gq in range(1, 5):
                            P.op('dve', lambda e, j=j, dsl=dsl, i=i, gq=gq: e.scalar_tensor_tensor(out=acc[j][:, dsl], in0=py[gq][:, :], scalar=rs_all[:, gq, i:i + 1], in1=acc[j][:, dsl], op0=ALU.mult, op1=ALU.add),
                                 [('py', gq), 'rs_all', ('acc', j)], [('acc', j)])
                    P.op('dve', lambda e, j=j: e.scalar_tensor_tensor(out=acc[j][:], in0=xt[j][:], scalar=DN_ALPHA, in1=acc[j][:], op0=ALU.mult, op1=ALU.add), [('xt', j), ('acc', j)], [('acc', j)])
                    layernorm(acc[j], ('acc', j), lnr1, x1[j], ('x1', j))
                    P.dma('sp', lambda e, j=j, rsl=rsl: e.dma_start(out=x1_d[rsl, :], in_=x1[j][:]), [('x1', j)], [('x1_d', i)], 'wx1%d' % j)
                    P.op('act', lambda e, j=j: e.activation(out=x1b[j][:], in_=x1[j][:], func=AF.Copy), [('x1', j)], [('x1b', j)])
                    P.dma('sp', lambda e, j=j, rsl=rsl: e.dma_start(out=x1b_d[rsl, :], in_=x1b[j][:]), [('x1b', j)], [('x1b_d', i)], 'wx1b%d' % j)
                    for k in range(8):
                        P.op('pe', lambda e, k=k, j=j: e.transpose(out=pT[:, k * 128:(k + 1) * 128], in_=x1[j][:, k * 128:(k + 1) * 128], identity=ident), [('x1', j), 'cst'], ['pT'])
                    P.op('act', lambda e, j=j: e.activation(out=x1T[j][:], in_=pT[:, :], func=AF.Copy), ['pT'], [('x1T', j)])
                    for k in range(8):
                        P.op('pe', lambda e, k=k, j=j: e.matmul(out=plg[:, 0:NE], lhsT=x1T[j][:, k * 128:(k + 1) * 128], rhs=rw[:, k, :], start=(k == 0), stop=(k == 7)), [('x1T', j), 'rw'], ['plg'])
                    P.op('dve', lambda e, i=i: e.tensor_add(out=lg_all[:, i, :], in0=plg[:, 0:NE], in1=rb[:]), ['plg', 'rb'], ['lg_all'])
                P.barrier()

            with ExitStack() as ph:
                m8 = sb("rm8", [128, NT, 8], F32, ph)
                wsum = sb("rwsum", [128, NT], F32, ph)
                M = sb("rM", [128, NT, NE], BF16, ph)
                Msum = sb("rMsum", [128, NE], F32, ph)
                Msb = sb("rMsb", [128, NE], BF16, ph)
                rank = sb("rrank", [128, NT, NE], F32, ph)
                tmp3 = sb("rtmp3", [128, NT, NE], F32, ph)
                ustrb = sb("rustrb", [128, 128], BF16, ph)
                cnt = sb("rcnt", [128, NE], F32, ph)
                pad = sb("rpad", [128, NE], F32, ph)
                pend = sb("rpend", [128, NE], F32, ph)
                pstart = sb("rpst", [128, NE], F32, ph)
                destf = sb("rdestf", [128, NT, 4], F32, ph)
                cmp3 = sb("rcmp3", [128, NB, NE], F32, ph)
                ebf = sb("rebf", [128, NB], F32, ph)
                basef = sb("rbasef", [128, NB], F32, ph)
                tmpb = sb("rtmpb", [128, NB], F32, ph)
                xs = [sb("rxs%d" % j, [128, D], BF16, ph) for j in range(2)]
                prk = [ps("rprk%d" % j, [128, 512], F32, ph) for j in range(2)]

                P.op('dve', lambda e: e.tensor_copy(out=ustrb[:], in_=cst[:, 481:609]), ['cst'], ['ustrb'])
                for i in range(NT):
                    P.op('dve', lambda e, i=i: e.max(out=m8[:, i, :], in_=lg_all[:, i, :]), ['lg_all'], ['m8'])
                P.op('dve', lambda e: e.tensor_tensor(out=M[:], in0=lg_all[:], in1=m8[:, :, 3:4].to_broadcast([128, NT, NE]), op=ALU.is_ge), ['lg_all', 'm8'], ['M'])
                P.op('dve', lambda e: e.tensor_sub(out=wk[:], in0=m8[:, :, 0:4], in1=m8[:, :, 0:1].to_broadcast([128, NT, 4])), ['m8'], ['wk'])
                P.op('act', lambda e: e.activation(out=wk[:], in_=wk[:], func=AF.Exp), ['wk'], ['wk'])
                P.op('dve', lambda e: e.reduce_sum(out=wsum[:], in_=wk[:], axis=AX.X), ['wk'], ['wsum'])
                P.op('dve', lambda e: e.reciprocal(out=wsum[:], in_=wsum[:]), ['wsum'], ['wsum'])
                P.op('dve', lambda e: e.tensor_mul(out=wk[:], in0=wk[:], in1=wsum[:].unsqueeze(2).to_broadcast([128, NT, 4])), ['wk', 'wsum'], ['wk'])
                P.op('dve', lambda e: e.memset(Msum[:], 0.0), [], ['Msum'])
                for i in range(NT):
                    pj = i % 2
                    P.op('pe', lambda e, i=i, pj=pj: e.matmul(out=prk[pj][:, 0:NE], lhsT=ustrb[:], rhs=M[:, i, :], start=True, stop=(i == 0)), ['ustrb', 'M'], [('prk', pj)])
                    if i > 0:
                        P.op('pe', lambda e, pj=pj: e.matmul(out=prk[pj][:, 0:NE], lhsT=ones_b[:, 0:128], rhs=Msb[:], start=False, stop=True), ['ones_b', 'Msb'], [('prk', pj)])
                    P.op('dve', lambda e, i=i, pj=pj: e.tensor_copy(out=rank[:, i, :], in_=prk[pj][:, 0:NE]), [('prk', pj)], ['rank'])
                    P.op('dve', lambda e, i=i: e.tensor_add(out=Msum[:], in0=Msum[:], in1=M[:, i, :]), ['Msum', 'M'], ['Msum'])
                    P.op('dve', lambda e: e.tensor_copy(out=Msb[:], in_=Msum[:]), ['Msum'], ['Msb'])
                P.op('pe', lambda e: e.matmul(out=prk[0][:, 64:64 + NE], lhsT=ones_b[:, 0:128], rhs=Msb[:], start=True, stop=True), ['ones_b', 'Msb'], [('prk', 0)])
                P.op('dve', lambda e: e.tensor_copy(out=cnt[:], in_=prk[0][:, 64:64 + NE]), [('prk', 0)], ['cnt'])
                P.op('dve', lambda e: e.memset(pad[:], 0.0), [], ['pad'])
                for m in range(NT // 4 + 1):
                    P.op('dve', lambda e, m=m: e.scalar_tensor_tensor(out=pad[:], in0=cnt[:], scalar=512.0 * m, in1=pad[:], op0=ALU.is_gt, op1=ALU.add), ['cnt', 'pad'], ['pad'])
                P.op('dve', lambda e: e.tensor_scalar(out=pad[:], in0=pad[:], scalar1=512.0, scalar2=None, op0=ALU.mult), ['pad'], ['pad'])
                P.op('dve', lambda e: e.tensor_tensor_scan(out=pend[:], data0=ones_f[:, 0:NE], data1=pad[:], initial=0.0, op0=ALU.mult, op1=ALU.add), ['pad', 'ones_f'], ['pend'])
                P.op('dve', lambda e: e.tensor_sub(out=pstart[:], in0=pend[:], in1=pad[:]), ['pend', 'pad'], ['pstart'])
                P.op('dve', lambda e: e.tensor_add(out=rank[:], in0=rank[:], in1=pstart[:].unsqueeze(1).to_broadcast([128, NT, NE])), ['rank', 'pstart'], ['rank'])
                for k in range(4):
                    P.op('dve', lambda e, k=k: e.tensor_tensor(out=tmp3[:], in0=lg_all[:], in1=m8[:, :, k:k + 1].to_broadcast([128, NT, NE]), op=ALU.is_equal), ['lg_all', 'm8'], ['tmp3'])
                    P.op('dve', lambda e: e.tensor_mul(out=tmp3[:], in0=tmp3[:], in1=rank[:]), ['tmp3', 'rank'], ['tmp3'])
                    P.op('dve', lambda e, k=k: e.reduce_sum(out=destf[:, :, k], in_=tmp3[:], axis=AX.X), ['tmp3'], ['destf'])
                P.op('dve', lambda e: e.tensor_copy(out=desti[:], in_=destf[:]), ['destf'], ['desti'])
                P.op('dve', lambda e: e.tensor_tensor(out=cmp3[:], in0=pend[:].unsqueeze(1).to_broadcast([128, NB, NE]), in1=thr[:, 0:NB].unsqueeze(2).to_broadcast([128, NB, NE]), op=ALU.is_le), ['pend', 'cst'], ['cmp3'])
                P.op('dve', lambda e: e.reduce_sum(out=ebf[:], in_=cmp3[:], axis=AX.X), ['cmp3'], ['ebf'])
                P.op('dve', lambda e: e.tensor_scalar(out=ebf[:], in0=ebf[:], scalar1=float(NE - 1), scalar2=float(l * NE), op0=ALU.min, op1=ALU.add), ['ebf'], ['ebf'])
                P.op('dve', lambda e: e.tensor_copy(out=bidx[:], in_=ebf[:]), ['ebf'], ['bidx'])
                P.op('dve', lambda e: e.tensor_scalar(out=tmpb[:], in0=ebf[:], scalar1=16.0, scalar2=cst[:, 609:610], op0=ALU.mult, op1=ALU.add), ['ebf', 'cst'], ['tmpb'])
                P.op('dve', lambda e: e.tensor_copy(out=bidx16[:], in_=tmpb[:]), ['tmpb'], ['bidx16'])
                P.op('dve', lambda e: e.tensor_scalar(out=basef[:], in0=ebf[:], scalar1=1024.0, scalar2=iota_p, op0=ALU.mult, op1=ALU.add), ['ebf', 'cst'], ['basef'])
                for k in range(8):
                    P.op('dve', lambda e, k=k: e.tensor_scalar(out=widx[:, 2, :, k], in0=basef[:], scalar1=float(k * 128), scalar2=None, op0=ALU.add), ['basef'], ['widx'])
                    P.op('dve', lambda e, k=k: e.tensor_scalar(out=tmpb[:], in0=basef[:], scalar1=float(k * 128), scalar2=2.0, op0=ALU.add, op1=ALU.mult), ['basef'], ['tmpb'])
                    P.op('dve', lambda e, k=k: e.tensor_copy(out=widx[:, 0, :, k], in_=tmpb[:]), ['tmpb'], ['widx'])
                    P.op('dve', lambda e, k=k: e.tensor_scalar(out=widx[:, 1, :, k], in0=tmpb[:], scalar1=1.0, scalar2=None, op0=ALU.add), ['tmpb'], ['widx'])
                for i in range(NT):
                    j = i % 2
                    P.dma('sp', lambda e, i=i, j=j: e.dma_start(out=xs[j][:], in_=x1b_d[i * 128:(i + 1) * 128, :]), [], [('xs', j)], 'rxsl%d' % j)
                    for k in range(4):
                        P.dma('pool', lambda e, i=i, j=j, k=k: e.indirect_dma_start(out=xg_d, out_offset=bass.IndirectOffsetOnAxis(ap=desti[:, i, k:k + 1], axis=0), in_=xs[j][:], in_offset=None),
                              [('xs', j), 'desti'], [('xg', i, k)], 'rxs%d' % j)
                P.barrier()

            with ExitStack() as ph:
                wsl = [sb("fw%d" % j, [128, 8, D], BF16, ph) for j in range(6)]
                bg16 = [sb("fbg%d" % j, [128, 128], F32, ph) for j in range(2)]
                bgT = [sb("fbgT%d" % j, [128, 16], F32, ph) for j in range(2)]
                bdn = [sb("fbd%d" % j, [128, D], F32, ph) for j in range(2)]
                xgt = [sb("fxg%d" % j, [128, 4, D], BF16, ph) for j in range(2)]
                xgT = [sb("fxgT%d" % j, [128, 8, 512], BF16, ph) for j in range(2)]
                hid = sb("fhid", [128, 8, 512], BF16, ph)
                tg = [sb("ftg%d" % j, [128, 512], F32, ph) for j in range(2)]
                ts = [sb("fts%d" % j, [128, 512], F32, ph) for j in range(2)]
                tu = [sb("ftu%d" % j, [128, 512], F32, ph) for j in range(2)]
                yt = [sb("fyt%d" % j, [128, D], F32, ph) for j in range(2)]
                pxt = [ps("fpx%d" % j, [128, 1024], BF16, ph) for j in range(2)]
                pg = [ps("fpg%d" % j, [128, 512], F32, ph) for j in range(2)]
                pu = [ps("fpu%d" % j, [128, 512], F32, ph) for j in range(2)]
                pd = [ps("fpd%d" % j, [128, 512], F32, ph) for j in range(2)]
                wgu2 = w_gu.rearrange("r (h c) -> (r h) c", h=2)
                wsrc = [wgu2, wgu2, w_dn]
                bgu16 = b_gu.rearrange("r (c q) -> (r c) q", q=128)
                nyt = [0]

                def f_load(b):
                    j = b % 2
                    for q in range(3):
                        sl = (3 * b + q) % 6
                        for k in range(8):
                            P.dma('pool', lambda e, q=q, k=k, sl=sl: e.indirect_dma_start(out=wsl[sl][:, k, :], out_offset=None, in_=wsrc[q],
                                  in_offset=bass.IndirectOffsetOnAxis(ap=widx[:, q, b, k:k + 1], axis=0)), ['widx'], [('wsl', sl)], 'fw%d' % sl)
                    P.dma('pool', lambda e: e.indirect_dma_start(out=bg16[j][:], out_offset=None, in_=bgu16, in_offset=bass.IndirectOffsetOnAxis(ap=bidx16[:, b:b + 1], axis=0)), ['bidx16'], [('bg16', j)], 'fbg%d' % j)
                    P.dma('pool', lambda e: e.indirect_dma_start(out=bdn[j][:], out_offset=None, in_=b_dn, in_offset=bass.IndirectOffsetOnAxis(ap=bidx[:, b:b + 1], axis=0)), ['bidx'], [('bdn', j)], 'fbd%d' % j)
                    P.dma('sp', lambda e: e.dma_start(out=xgt[j][:], in_=xg_d[b * 512:(b + 1) * 512, :].rearrange("(r p) d -> p r d", p=128)), [], [('xgt', j)], 'fxg%d' % j)

                def f_transposes(b):
                    j = b % 2
                    for dk in range(8):
                        pj = dk % 2
                        for r in range(4):
                            P.op('pe', lambda e, dk=dk, r=r, pj=pj: e.transpose(out=pxt[pj][:, r * 128:(r + 1) * 128], in_=xgt[j][:, r, dk * 128:(dk + 1) * 128], identity=identb[:]),
                                 [('xgt', j), 'identb'], [('pxt', pj)])
                        P.op('act', lambda e, dk=dk, pj=pj: e.activation(out=xgT[j][:, dk, :], in_=pxt[pj][:, 0:512], func=AF.Copy), [('pxt', pj)], [('xgT', j)])
                    P.op('pe', lambda e: e.transpose(out=pd[0][:, 0:128], in_=bg16[j][:, :], identity=ident), [('bg16', j), 'cst'], [('pd', 0)])
                    P.op('act', lambda e: e.activation(out=bgT[j][:], in_=pd[0][:, 0:16], func=AF.Copy), [('pd', 0)], [('bgT', j)])

                def f_gateup(b):
                    j = b % 2
                    sG, sU = (3 * b) % 6, (3 * b + 1) % 6
                    for jc in range(8):
                        pj = jc % 2
                        for (sl, pp, ppk) in ((sG, pg[pj], ('pg', pj)), (sU, pu[pj], ('pu', pj))):
                            for k in range(8):
                                P.op('pe', lambda e, sl=sl, pp=pp, k=k, jc=jc: e.matmul(out=pp[:, :], lhsT=wsl[sl][:, k, jc * 128:(jc + 1) * 128], rhs=xgT[j][:, k, :], start=(k == 0), stop=(k == 7)),
                                     [('wsl', sl), ('xgT', j)], [ppk])
                        P.op('dve', lambda e, pj=pj, jc=jc: e.tensor_scalar(out=tg[pj][:], in0=pg[pj][:, :], scalar1=bgT[j][:, jc:jc + 1], scalar2=7.0, op0=ALU.add, op1=ALU.min), [('pg', pj), ('bgT', j)], [('tg', pj)])
                        P.op('act', lambda e, pj=pj: e.activation(out=ts[pj][:], in_=tg[pj][:], func=AF.Sigmoid, scale=1.702), [('tg', pj)], [('ts', pj)])
                        P.op('act', lambda e, pj=pj, jc=jc: e.activation(out=tu[pj][:], in_=pu[pj][:, :], func=AF.Identity, bias=bgT[j][:, 8 + jc:9 + jc]), [('pu', pj), ('bgT', j)], [('tu', pj)])
                        P.op('dve', lambda e, pj=pj: e.tensor_mul(out=tg[pj][:], in0=tg[pj][:], in1=ts[pj][:]), [('tg', pj), ('ts', pj)], [('tg', pj)])
                        P.op('dve', lambda e, pj=pj: e.tensor_scalar(out=tu[pj][:], in0=tu[pj][:], scalar1=7.0, scalar2=-7.0, op0=ALU.min, op1=ALU.max), [('tu', pj)], [('tu', pj)])
                        P.op('dve', lambda e, pj=pj, jc=jc: e.scalar_tensor_tensor(out=hid[:, jc, :], in0=tu[pj][:], scalar=1.0, in1=tg[pj][:], op0=ALU.add, op1=ALU.mult), [('tu', pj), ('tg', pj)], ['hid'])

                def f_down(b):
                    j = b % 2
                    sD = (3 * b + 2) % 6
                    for rt in range(4):
                        yj = nyt[0] % 2
                        nyt[0] += 1
                        for dh in range(2):
                            for jc in range(8):
                                P.op('pe', lambda e, rt=rt, dh=dh, jc=jc: e.matmul(out=pd[dh][:, :], lhsT=hid[:, jc, rt * 128:(rt + 1) * 128], rhs=wsl[sD][:, jc, dh * 512:(dh + 1) * 512], start=(jc == 0), stop=(jc == 7)),
                                     ['hid', ('wsl', sD)], [('pd', dh)])
                            P.op('dve', lambda e, dh=dh, yj=yj: e.tensor_add(out=yt[yj][:, dh * 512:(dh + 1) * 512], in0=pd[dh][:, :], in1=bdn[j][:, dh * 512:(dh + 1) * 512]), [('pd', dh), ('bdn', j)], [('yt', yj)])
                        P.dma('sp', lambda e, rt=rt, yj=yj: e.dma_start(out=yg_d[b * 512 + rt * 128:b * 512 + (rt + 1) * 128, :], in_=yt[yj][:]), [('yt', yj)], [('yg', b, rt)], 'fyt%d' % yj)

                f_load(0)
                f_transposes(0)
                for b in range(NB):
                    if b + 1 < NB:
                        f_load(b + 1)
                    f_gateup(b)
                    if b + 1 < NB:
                        f_transposes(b + 1)
                    f_down(b)
                P.barrier()

            with ExitStack() as ph:
                lnr2 = load_lnr(ph, 2)
                gk = [sb("cgk%d" % j, [128, 4, D], F32, ph) for j in range(2)]
                x1t = [sb("cx1%d" % j, [128, D], F32, ph) for j in range(2)]
                x2 = [sb("cx2%d" % j, [128, D], F32, ph) for j in range(2)]
                xTb = [sb("cxb%d" % j, [128, 8, 128], BF16, ph) for j in range(2)]
                pT2 = [ps("cpT%d" % j, [128, 1024], F32, ph) for j in range(2)]
                for i in range(NT):
                    j = i % 2
                    rsl = slice(i * 128, (i + 1) * 128)
                    for k in range(4):
                        P.dma('pool', lambda e, i=i, j=j, k=k: e.indirect_dma_start(out=gk[j][:, k, :], out_offset=None, in_=yg_d, in_offset=bass.IndirectOffsetOnAxis(ap=desti[:, i, k:k + 1], axis=0)),
                              ['desti'], [('gk', j)], 'cgk%d' % j)
                    P.dma('sp', lambda e, j=j, rsl=rsl: e.dma_start(out=x1t[j][:], in_=x1_d[rsl, :]), [], [('x1t', j)], 'cx1%d' % j)
                    P.op('dve', lambda e, j=j, i=i: e.scalar_tensor_tensor(out=x1t[j][:], in0=x1t[j][:], scalar=DN_ALPHA, in1=x1t[j][:], op0=ALU.mult, op1=ALU.bypass) if False else
                         e.tensor_scalar(out=x1t[j][:], in0=x1t[j][:], scalar1=DN_ALPHA, scalar2=None, op0=ALU.mult), [('x1t', j)], [('x1t', j)])
                    for k in range(4):
                        P.op('dve', lambda e, j=j, i=i, k=k: e.scalar_tensor_tensor(out=x1t[j][:], in0=gk[j][:, k, :], scalar=wk[:, i, k:k + 1], in1=x1t[j][:], op0=ALU.mult, op1=ALU.add),
                             [('gk', j), 'wk', ('x1t', j)], [('x1t', j)])
                    layernorm(x1t[j], ('x1t', j), lnr2, x2[j], ('x2', j))
                    P.dma('sp', lambda e, j=j, rsl=rsl: e.dma_start(out=xnext[rsl, :], in_=x2[j][:]), [('x2', j)], [('xres', i)], 'cx2%d' % j)
                    if l < L - 1:
                        emit_xT(ph, x2[j], ('x2', j), i, pT2[j], xTb[j], j)
                P.barrier()
    es.close()
    return nc


def make_consts():
    c = np.zeros((128, NCON), np.float32)
    c[:, 0:128] = np.eye(128, dtype=np.float32)
    s = np.arange(32)[:, None]
    t = np.arange(32)[None, :]
    c[0:32, 128:256] = np.tile((s <= t).astype(np.float32), (1, 4))
    c[0:32, 256:384] = np.tile((s >= t).astype(np.float32), (1, 4))
    c[:, 384] = np.arange(128)
    c[:, 385:449] = (np.arange(64) * 512)[None, :]
    c[:, 449:481] = np.arange(32)[None, :]
    c[:, 481:609] = (np.arange(128)[:, None] < np.arange(128)[None, :]).astype(np.float32)
    c[:, 609] = np.arange(128) % 16
    return c


def pack_inputs(inp, L):
    f = lambda a: np.ascontiguousarray(np.asarray(a, dtype=np.float32))
    smallp = np.concatenate([
        f(inp["hg_lb"]).reshape(L * 8, 128), f(inp["hg_norm"]).reshape(L * 4, 128),
        f(inp["lru_conv_w"]).reshape(L * 16, 128), f(inp["lru_conv_b"]).reshape(L * 4, 128),
        f(inp["lru_ba"]).reshape(L * 8, 128), f(inp["lru_bx"]).reshape(L * 8, 128),
        f(inp["lru_lambda"]).reshape(L * 8, 128), f(inp["lru_norm"]).reshape(L * 4, 128)], axis=0)
    lnp = np.stack([f(inp["ln1_g"]), f(inp["ln1_b"]), f(inp["ln2_g"]), f(inp["ln2_b"])], axis=1).reshape(L * 4, D)
    return {
        "w_in": f(inp["w_in"]).reshape(L * D, 3584),
        "smallp": np.ascontiguousarray(smallp),
        "lru_wa": f(inp["lru_wa"]).reshape(L * 2 * 8 * 64, 64),
        "lru_wx": f(inp["lru_wx"]).reshape(L * 2 * 8 * 64, 64),
        "w_out": f(inp["w_out"]).reshape(L * D, D),
        "lnp": np.ascontiguousarray(lnp),
        "router_w": f(inp["router_w"]).reshape(L * D, NE),
        "router_b": f(inp["router_b"]).reshape(L, NE),
        "w_gate_up": f(inp["w_gate_up"]).reshape(L * NE * D, 2 * D),
        "b_gate_up": f(inp["b_gate_up"]).reshape(L * NE, 2 * D),
        "w_down": f(inp["w_down"]).reshape(L * NE * D, D),
        "b_down": f(inp["b_down"]).reshape(L * NE, D),
        "consts": make_consts(),
    }


def run(inp, n_cores, S, L, dbg=False):
    nc = build_nc(S, L, dbg)
    shared = pack_inputs(inp, L)
    x = np.asarray(inp["x"], dtype=np.float32)
    in_maps = []
    for c in range(n_cores):
        m = dict(shared)
        m["x"] = np.ascontiguousarray(x[c])
        in_maps.append(m)
    res = run_bass_kernel_spmd(nc, in_maps, core_ids=list(range(n_cores)))
    return res


def kernel(**inputs):
    res = run(inputs, 8, 4096, 4)
    return np.stack([r["out"] for r in res.results], axis=0).astype(np.float32)
```

```python
from contextlib import ExitStack
import numpy as np
import concourse.bass as bass
import concourse.mybir as mybir
from concourse.bass_utils import run_bass_kernel_spmd

F32 = mybir.dt.float32
BF16 = mybir.dt.bfloat16
I32 = mybir.dt.int32
ALU = mybir.AluOpType
AF = mybir.ActivationFunctionType
AX = mybir.AxisListType

D = 1024
NE = 32
DN_ALPHA = float(8 ** 0.25)
LN_EPS = 1e-5
RMS_EPS = 1e-6
import os
SAME_ENGINE_SYNC = set(os.environ.get('K_SES', 'dve,pool,sp').split(','))
NCON = 128 + 128 + 128 + 1 + 64 + 32 + 128 + 1


class Prog:
    ENG = ('pe', 'act', 'dve', 'pool', 'sp')

    def __init__(self, nc, es):
        self.nc = nc
        self.es = es
        self.E = {'pe': nc.tensor, 'act': nc.scalar, 'dve': nc.vector, 'pool': nc.gpsimd, 'sp': nc.sync}
        self.sem = {e: es.enter_context(nc.semaphore("c_" + e)) for e in self.ENG}
        self.cnt = {e: 0 for e in self.ENG}
        self.known = {e: {} for e in self.ENG}
        self.bufs = {}
        self.lanes = {}
        self.semname = {}
        for e in self.ENG:
            self.semname[id(self.sem[e])] = e

    def lane(self, name):
        if name not in self.lanes:
            s = self.es.enter_context(self.nc.semaphore("l_%d" % len(self.lanes)))
            self.lanes[name] = [s, 0]
        return self.lanes[name]

    BIG = set(os.environ.get('K_BIG', 'tg,ts,tu,hid,yt,acc,x1,x1t,kk,E,A,Bk,Kh,q,v,sg,xc,zx,xcb,gy,t1,t2').split(','))

    def _deps(self, eng, r, w, skip_sem=None):
        deps = {}
        def add(ev, b=None):
            if ev is None:
                return
            s, v = ev
            if s is skip_sem:
                return
            if s is self.sem[eng] and eng not in SAME_ENGINE_SYNC:
                return
            if s is self.sem[eng] and eng == 'dve' and b is not None:
                nm = b[0] if isinstance(b, tuple) else b
                if nm in self.BIG:
                    return
            k = id(s)
            if k not in deps or deps[k][1] < v:
                deps[k] = (s, v)
        for b in r:
            st = self.bufs.get(b)
            if st:
                add(st[0], b)
        for b in w:
            st = self.bufs.get(b)
            if st:
                add(st[0], b)
                for ev in st[1]:
                    add(ev, b)
        kn = self.known[eng]
        for k, (s, v) in deps.items():
            if kn.get(k, 0) >= v:
                continue
            self.E[eng].wait_ge(s, v)
            kn[k] = v

    def _mark(self, ev, r, w):
        for b in r:
            st = self.bufs.setdefault(b, [None, []])
            st[1].append(ev)
            if len(st[1]) > 24:
                best = {}
                for s, v in st[1]:
                    if id(s) not in best or best[id(s)][1] < v:
                        best[id(s)] = (s, v)
                st[1] = list(best.values())
        for b in w:
            self.bufs[b] = [ev, []]

    def op(self, eng, fn, r=(), w=()):
        self._deps(eng, r, w)
        ins = fn(self.E[eng])
        self.cnt[eng] += 1
        ins.then_inc(self.sem[eng], 1)
        self._mark((self.sem[eng], self.cnt[eng]), r, w)

    def dma(self, q, fn, r, w, lane):
        ln = self.lane(lane)
        self._deps(q, r, w, skip_sem=ln[0])
        ins = fn(self.E[q])
        ln[1] += 16
        ins.then_inc(ln[0], 16)
        self._mark((ln[0], ln[1]), r, w)

    def barrier(self):
        for e in self.ENG:
            kn = self.known[e]
            for e2 in self.ENG:
                if e2 == e or self.cnt[e2] == 0:
                    continue
                s = self.sem[e2]
                if kn.get(id(s), 0) < self.cnt[e2]:
                    self.E[e].wait_ge(s, self.cnt[e2])
                    kn[id(s)] = self.cnt[e2]
            for name, (s, v) in self.lanes.items():
                if v and kn.get(id(s), 0) < v:
                    self.E[e].wait_ge(s, v)
                    kn[id(s)] = v
        self.bufs = {}


def build_nc(S, L, dbg=False):
    NT = S // 128
    NTB = S // 512
    NCH = S // 32
    NB = (4 * S + NE * 511) // 512
    NR = NB * 512
    nc = bass.Bass("TRN2", target_bir_lowering=False)

    def din(name, shape, dt=F32):
        return nc.dram_tensor(name, shape, dt, kind="ExternalInput").ap()

    x_in = din("x", [S, D])
    w_in = din("w_in", [L * D, 3584])
    smallp = din("smallp", [60 * L, 128])
    lru_wa = din("lru_wa", [L * 2 * 8 * 64, 64])
    lru_wx = din("lru_wx", [L * 2 * 8 * 64, 64])
    w_out = din("w_out", [L * D, D])
    lnp = din("lnp", [L * 4, D])
    router_w = din("router_w", [L * D, NE])
    router_b = din("router_b", [L, NE])
    w_gu = din("w_gate_up", [L * NE * D, 2 * D])
    b_gu = din("b_gate_up", [L * NE, 2 * D])
    w_dn = din("w_down", [L * NE * D, D])
    b_dn = din("b_down", [L * NE, D])
    consts = din("consts", [128, NCON])
    out = nc.dram_tensor("out", [S, D], F32, kind="ExternalOutput").ap()

    def dscr(name, shape, dt):
        kind = "ExternalOutput" if dbg else "Internal"
        return nc.dram_tensor(name, shape, dt, kind=kind).ap()

    xT_d = dscr("xT_d", [8, 128, S], BF16)
    mixT_d = dscr("mixT_d", [8, 128, S], BF16)
    xres_d = dscr("xres_d", [S, D], F32)
    x1_d = dscr("x1_d", [S, D], F32)
    x1b_d = dscr("x1b_d", [S, D], BF16)
    xg_d = dscr("xg_d", [NR, D], BF16)
    yg_d = dscr("yg_d", [NR, D], F32)

    es = ExitStack()
    P = Prog(nc, es)
    dumped = set()

    def dump(name, ap, shape, dt, keys):
        if not dbg or name in dumped:
            return
        dumped.add(name)
        d = nc.dram_tensor("D_" + name, shape, dt, kind="ExternalOutput").ap()
        P.dma('sp', lambda e: e.dma_start(out=d, in_=ap), keys, [('dump', name)], 'dump_' + name)

    uid = [0]

    def sb(name, shape, dt, st=es):
        uid[0] += 1
        return st.enter_context(nc.sbuf_tensor("%s_%d" % (name, uid[0]), shape, dt))

    def ps(name, shape, dt, st=es):
        uid[0] += 1
        return st.enter_context(nc.psum_tensor("%s_%d" % (name, uid[0]), shape, dt))

    cst = sb("cst", [128, NCON], F32)
    ident = cst[:, 0:128]
    tri = [cst[0:32, 128:256], cst[0:32, 256:384]]
    iota_p = cst[:, 384:385]
    thr = cst[:, 385:385 + 64]
    iota_e = cst[:, 449:449 + 32]
    identb = sb("identb", [128, 128], BF16)
    ones_f = sb("ones_f", [128, 128], F32)
    ones_b = sb("ones_b", [128, 512], BF16)
    cmask = sb("cmask", [128, S], BF16)
    SP = sb("SP", [128, 60 * L], F32)
    LOW = sb("LOW", [128, L * 8], F32)
    OML = sb("OML", [128, L * 8], F32)
    CP = sb("CP", [128, L * 8], F32)
    CP2 = sb("CP2", [128, L * 8], F32)
    ssq_all = sb("ssq_all", [128, 8, NT], F32)
    rs_all = sb("rs_all", [128, 5, NT], F32)

    P.dma('sp', lambda e: e.dma_start(out=cst[:], in_=consts), [], ['cst'], 'cst')
    P.op('dve', lambda e: e.tensor_copy(out=identb[:], in_=ident), ['cst'], ['identb'])
    P.op('dve', lambda e: e.memset(ones_f[:], 1.0), [], ['ones_f'])
    P.op('dve', lambda e: e.memset(ones_b[:], 1.0), [], ['ones_b'])
    P.op('dve', lambda e: e.memset(cmask[:], 1.0), [], ['cmask'])
    P.op('dve', lambda e: e.memset(cmask[:].rearrange("p (c j) -> p c j", j=32)[:, :, 0:1], 0.0), [], ['cmask'])

    off_lb, off_hn, off_cw, off_cb = 0, L * 8, L * 12, L * 28
    off_ba, off_bx, off_lam, off_ln = L * 32, L * 40, L * 48, L * 56
    with ExitStack() as ph:
        stg = sb("stg", [128, 128], F32, ph)
        pst = ps("pst", [128, 512], F32, ph)
        R = 60 * L
        r0 = 0
        while r0 < R:
            n = min(128, R - r0)
            P.dma('sp', lambda e, r0=r0, n=n: e.dma_start(out=stg[0:n, :], in_=smallp[r0:r0 + n, :]), [], ['stg'], 'stg')
            P.op('pe', lambda e, n=n: e.transpose(out=pst[:, 0:n], in_=stg[0:n, :], identity=ident[0:n, 0:n]), ['stg', 'cst'], ['pst'])
            P.op('dve', lambda e, r0=r0, n=n: e.tensor_copy(out=SP[:, r0:r0 + n], in_=pst[:, 0:n]), ['pst'], ['SP'])
            r0 += n
        tmp = sb("lbtmp", [128, L * 8], F32, ph)
        mx = sb("lbmx", [128, 8], F32, ph)
        sm = sb("lbsm", [128, 8], F32, ph)
        lb = SP[:, off_lb:off_lb + L * 8]
        P.op('dve', lambda e: e.tensor_copy(out=mx[:], in_=lb[:, 0:8]), ['SP'], ['mx'])
        for l in range(1, L):
            P.op('dve', lambda e, l=l: e.tensor_max(out=mx[:], in0=mx[:], in1=lb[:, l * 8:(l + 1) * 8]), ['SP', 'mx'], ['mx'])
        for l in range(L):
            P.op('dve', lambda e, l=l: e.tensor_sub(out=tmp[:, l * 8:(l + 1) * 8], in0=lb[:, l * 8:(l + 1) * 8], in1=mx[:]), ['SP', 'mx'], ['lbtmp'])
        P.op('act', lambda e: e.activation(out=tmp[:], in_=tmp[:], func=AF.Exp), ['lbtmp'], ['lbtmp'])
        P.op('dve', lambda e: e.tensor_copy(out=sm[:], in_=tmp[:, 0:8]), ['lbtmp'], ['sm'])
        for l in range(1, L):
            P.op('dve', lambda e, l=l: e.tensor_add(out=sm[:], in0=sm[:], in1=tmp[:, l * 8:(l + 1) * 8]), ['lbtmp', 'sm'], ['sm'])
        P.op('dve', lambda e: e.reciprocal(out=sm[:], in_=sm[:]), ['sm'], ['sm'])
        P.op('dve', lambda e: e.memset(LOW[:], 0.0), [], ['LOW'])
        for l in range(1, L):
            P.op('dve', lambda e, l=l: e.tensor_mul(out=tmp[:, l * 8:(l + 1) * 8], in0=tmp[:, l * 8:(l + 1) * 8], in1=sm[:]), ['lbtmp', 'sm'], ['lbtmp'])
            P.op('dve', lambda e, l=l: e.tensor_add(out=LOW[:, l * 8:(l + 1) * 8], in0=LOW[:, (l - 1) * 8:l * 8], in1=tmp[:, l * 8:(l + 1) * 8]), ['lbtmp', 'LOW'], ['LOW'])
        P.op('dve', lambda e: e.tensor_scalar(out=LOW[:], in0=LOW[:], scalar1=0.0, scalar2=1.0 - 1e-6, op0=ALU.max, op1=ALU.min), ['LOW'], ['LOW'])
        P.op('dve', lambda e: e.tensor_scalar(out=OML[:], in0=LOW[:], scalar1=-1.0, scalar2=1.0, op0=ALU.mult, op1=ALU.add), ['LOW'], ['OML'])
        lam = SP[:, off_lam:off_lam + L * 8]
        P.op('act', lambda e: e.activation(out=CP[:], in_=lam, func=AF.Sigmoid), ['SP'], ['CP'])
        P.op('act', lambda e: e.activation(out=CP[:], in_=CP[:], func=AF.Ln), ['CP'], ['CP'])
        P.op('dve', lambda e: e.tensor_scalar(out=CP2[:], in0=CP[:], scalar1=16.0, scalar2=None, op0=ALU.mult), ['CP'], ['CP2'])
        P.op('dve', lambda e: e.tensor_scalar(out=CP[:], in0=CP[:], scalar1=8.0, scalar2=None, op0=ALU.mult), ['CP', 'CP2'], ['CP'])
        dump("SP", SP[:], [128, 60 * L], F32, ['SP'])
        dump("LOW", LOW[:], [128, L * 8], F32, ['LOW'])
        dump("CP", CP[:], [128, L * 8], F32, ['CP'])
        P.barrier()

    def emit_xT(st, src, srckey, i, pT, xTb, par):
        for k in range(8):
            P.op('pe', lambda e, k=k: e.transpose(out=pT[:, k * 128:(k + 1) * 128], in_=src[:, k * 128:(k + 1) * 128], identity=ident),
                 [srckey, 'cst'], [('pT', par)])
        P.op('act', lambda e: e.activation(out=xTb[:].rearrange("p k t -> p (k t)"), in_=pT[:, :], func=AF.Copy), [('pT', par)], [('xTb', par)])
        P.dma('sp', lambda e: e.dma_start(out=xT_d[:, :, i * 128:(i + 1) * 128].rearrange("k p t -> p k t"), in_=xTb[:]),
              [('xTb', par)], [('xT_d', i // 4)], 'xTb%d' % par)

    with ExitStack() as ph:
        xt = [sb("p0x%d" % j, [128, D], F32, ph) for j in range(2)]
        xTb = [sb("p0b%d" % j, [128, 8, 128], BF16, ph) for j in range(2)]
        pT = [ps("p0p%d" % j, [128, 1024], F32, ph) for j in range(2)]
        for i in range(NT):
            j = i % 2
            P.dma('sp', lambda e, i=i, j=j: e.dma_start(out=xt[j][:], in_=x_in[i * 128:(i + 1) * 128, :]), [], [('xt', j)], 'p0x%d' % j)
            emit_xT(ph, xt[j], ('xt', j), i, pT[j], xTb[j], j)
        P.barrier()

    for l in range(L):
        xcur = x_in if l == 0 else xres_d
        xnext = out if l == L - 1 else xres_d

        for h in range(4):
            with ExitStack() as ph:
                q = sb("hq", [128, S], BF16, ph)
                v = sb("hv", [128, S], BF16, ph)
                sg = sb("hsg", [128, S], BF16, ph)
                logf = sb("hlogf", [128, S], F32, ph)
                kk = sb("hkk", [128, S], BF16, ph)
                g = sb("hg", [128, S], F32, ph)
                Eb = sb("hE", [128, S], F32, ph)
                A2 = [sb("hA%d" % d, [128, S], BF16, ph) for d in range(2)]
                Bk2 = [sb("hBk%d" % d, [128, S], BF16, ph) for d in range(2)]
                Kh2 = [sb("hKh%d" % d, [128, S], BF16, ph) for d in range(2)]
                oT = sb("hoT", [128, S], F32, ph)
                wq = sb("hwq", [128, 8, 5 * 128], BF16, ph)
                xb = [sb("hxb%d" % j, [128, 8, 512], BF16, ph) for j in range(2)]
                t1 = [sb("ht1%d" % j, [128, 512], F32, ph) for j in range(2)]
                eref2 = [sb("heref%d" % d, [128, NCH], F32, ph) for d in range(2)]
                dec2 = [sb("hdec%d" % d, [128, NCH], F32, ph) for d in range(2)]
                c22 = [sb("hc2%d" % d, [128, NCH], F32, ph) for d in range(2)]
                Sf2 = [sb("hSf%d" % d, [128, 128], F32, ph) for d in range(2)]
                Sb2 = [sb("hSb%d" % d, [128, 128], BF16, ph) for d in range(2)]
                scm2 = [[sb("hscm%d%d" % (d, j), [32, 128], BF16, ph) for j in range(2)] for d in range(2)]
                TT2 = [[sb("hTT%d%d" % (d, j), [32, 1024], BF16, ph) for j in range(2)] for d in range(2)]
                pz = [ps("hpz%d" % j, [128, 512], F32, ph) for j in range(2)]
                ps_sc1 = ps("hpsc", [128, 512], F32, ph)
                ps_t1 = ps("hpt", [128, 1024], BF16, ph)
                psk = [ps("hpk%d" % d, [128, 512], F32, ph) for d in range(2)]
                kcount = [0, 0]
                ps_o2 = [ps("hpo%d" % d, [128, 512], F32, ph) for d in range(2)]

                cols = [h * 128, 512 + h * 128, 1024 + h * 128, 1536 + h * 128, 2048 + h * 128]
                for z in range(5):
                    P.dma('pool', lambda e, z=z: e.dma_start(
                        out=wq[:, :, z * 128:(z + 1) * 128],
                        in_=w_in[l * D:(l + 1) * D, cols[z]:cols[z] + 128].rearrange("(k p) c -> p k c", p=128)),
                        [], ['wq'], 'wq')

                def project(zs):
                    for tb in range(NTB):
                        j = tb % 2
                        tsl = slice(tb * 512, (tb + 1) * 512)
                        P.dma('sp', lambda e, tb=tb, j=j: e.dma_start(out=xb[j][:], in_=xT_d[:, :, tb * 512:(tb + 1) * 512].rearrange("k p t -> p k t")),
                              [('xT_d', tb)], [('xb', j)], 'hxb%d' % j)
                        for zi, z in enumerate(zs):
                            pj = (tb * len(zs) + zi) % 2
                            for k in range(8):
                                P.op('pe', lambda e, k=k, z=z, j=j, pj=pj: e.matmul(out=pz[pj][:, :], lhsT=wq[:, k, z * 128:(z + 1) * 128], rhs=xb[j][:, k, :], start=(k == 0), stop=(k == 7)),
                                     ['wq', ('xb', j)], [('pz', pj)])
                            if z == 0:
                                P.op('act', lambda e, pj=pj, tsl=tsl: e.activation(out=q[:, tsl], in_=pz[pj][:, :], func=AF.Silu), [('pz', pj)], ['q'])
                            elif z == 1:
                                P.op('act', lambda e, pj=pj, tsl=tsl: e.activation(out=v[:, tsl], in_=pz[pj][:, :], func=AF.Copy), [('pz', pj)], ['v'])
                            elif z == 4:
                                P.op('act', lambda e, pj=pj, tsl=tsl: e.activation(out=sg[:, tsl], in_=pz[pj][:, :], func=AF.Silu), [('pz', pj)], ['sg'])
                            else:
                                P.op('act', lambda e, pj=pj, tsl=tsl: e.activation(out=logf[:, tsl], in_=pz[pj][:, :], func=AF.Sigmoid), [('pz', pj)], ['logf'])

                def gate_bulk(r):
                    col = l * 8 + r * 4 + h
                    P.op('dve', lambda e: e.tensor_scalar(out=logf[:], in0=logf[:], scalar1=OML[:, col:col + 1], scalar2=LOW[:, col:col + 1], op0=ALU.mult, op1=ALU.add),
                         ['logf', 'OML', 'LOW'], ['logf'])
                    P.op('dve', lambda e: e.tensor_scalar(out=kk[:], in0=logf[:], scalar1=-1.0, scalar2=1.0, op0=ALU.mult, op1=ALU.add), ['logf'], ['kk'])
                    P.op('act', lambda e: e.activation(out=logf[:], in_=logf[:], func=AF.Ln), ['logf', 'kk'], ['logf'])

                def prep(dr):
                    fwd = dr == 0
                    A, Bk, eref, dec, c2 = A2[dr], Bk2[dr], eref2[dr], dec2[dr], c22[dr]
                    V = (lambda ap: ap) if fwd else (lambda ap: ap[:, ::-1])
                    P.op('dve', lambda e: e.tensor_tensor_scan(out=V(g[:]), data0=cmask[:], data1=V(logf[:]), initial=0.0, op0=ALU.mult, op1=ALU.add),
                         ['cmask', 'logf'], ['g'])
                    g3 = g[:].rearrange("p (c j) -> p c j", j=32)
                    jm, jl = (15, 31) if fwd else (16, 0)
                    gref = g3[:, :, jm]
                    glast = g3[:, :, jl]
                    P.op('act', lambda e: e.activation(out=eref[:], in_=gref, func=AF.Exp), ['g'], [('eref', dr)])
                    P.op('act', lambda e: e.activation(out=dec[:], in_=glast, func=AF.Exp), ['g'], [('dec', dr)])
                    E3 = Eb[:].rearrange("p (c j) -> p c j", j=32)
                    P.op('dve', lambda e: e.tensor_sub(out=E3, in0=g3[:, :, jl:jl + 1].to_broadcast([128, NCH, 32]), in1=g3), ['g'], ['E'])
                    P.op('act', lambda e: e.activation(out=Eb[:], in_=Eb[:], func=AF.Exp), ['E'], ['E'])
                    P.op('dve', lambda e: e.tensor_mul(out=Kh2[dr][:], in0=kk[:], in1=Eb[:]), ['kk', 'E'], [('Kh', dr)])
                    P.op('dve', lambda e: e.tensor_sub(out=E3, in0=g3, in1=g3[:, :, jm:jm + 1].to_broadcast([128, NCH, 32])), ['g', 'E'], ['E'])
                    P.op('act', lambda e: e.activation(out=g[:], in_=Eb[:], func=AF.Exp), ['E', 'g', ('eref', dr), ('dec', dr)], ['g'])
                    P.op('act', lambda e: e.activation(out=Eb[:], in_=Eb[:], func=AF.Exp, scale=-1.0), ['E', 'g'], ['E'])
                    P.op('dve', lambda e: e.tensor_mul(out=A[:], in0=q[:], in1=g[:]), ['q', 'g'], [('A', dr)])
                    P.op('dve', lambda e: e.tensor_mul(out=Bk[:], in0=kk[:], in1=Eb[:]), ['kk', 'E'], [('Bk', dr)])
                    P.op('dve', lambda e: e.memset(Sf2[dr][:], 0.0), [], [('Sf', dr)])
                    P.op('dve', lambda e: e.memset(Sb2[dr][:], 0.0), [], [('Sb', dr)])

                def tile_head(dr, n_i, i):
                    A, Bk = A2[dr], Bk2[dr]
                    j = n_i % 2
                    scm, TT, ps_sc, ps_t = scm2[dr][j], TT2[dr][j], ps_sc1, ps_t1
                    for cc in range(4):
                        csl = slice((4 * i + cc) * 32, (4 * i + cc + 1) * 32)
                        P.op('pe', lambda e, cc=cc, csl=csl: e.matmul(out=ps_sc[0:32, cc * 32:(cc + 1) * 32], lhsT=Bk[:, csl], rhs=A[:, csl], start=True, stop=True),
                             [('Bk', dr), ('A', dr)], ['ps_sc'])
                    P.op('dve', lambda e: e.tensor_mul(out=scm[:], in0=ps_sc[0:32, 0:128], in1=tri[dr]), ['ps_sc', 'cst'], [('scm', dr, j)])
                    for cc in range(4):
                        csl = slice((4 * i + cc) * 32, (4 * i + cc + 1) * 32)
                        P.op('pe', lambda e, cc=cc, csl=csl: e.transpose(out=ps_t[0:32, cc * 128:(cc + 1) * 128], in_=Kh2[dr][:, csl], identity=identb[:]),
                             [('Kh', dr), 'identb'], ['ps_t'])
                    for cc in range(4):
                        csl = slice((4 * i + cc) * 32, (4 * i + cc + 1) * 32)
                        P.op('pe', lambda e, cc=cc, csl=csl: e.transpose(out=ps_t[0:32, 512 + cc * 128:512 + (cc + 1) * 128], in_=v[:, csl], identity=identb[:]),
                             ['v', 'identb'], ['ps_t'])
                    P.op('dve', lambda e: e.tensor_copy(out=TT[:], in_=ps_t[0:32, 0:1024]), ['ps_t'], [('TT', dr, j)])

                def emit_kv(dr, n_i, cc):
                    j = n_i % 2
                    TT = TT2[dr][j]
                    par = kcount[dr] % 2
                    kcount[dr] += 1
                    buf, key = (pz[dr], ('pz', dr)) if par == 0 else (psk[dr], ('psk', dr))
                    P.op('pe', lambda e: e.matmul(out=buf[:, 0:128], lhsT=TT[:, cc * 128:(cc + 1) * 128], rhs=TT[:, 512 + cc * 128:512 + (cc + 1) * 128], start=True, stop=True),
                         [('TT', dr, j)], [key])
                    return buf, key

                def chunk(dr, n_i, i, cc, kvb):
                    fwd = dr == 0
                    A = A2[dr]
                    eref, dec, Sf, Sb = eref2[dr], dec2[dr], Sf2[dr], Sb2[dr]
                    j = n_i % 2
                    scm, TT, ps_o = scm2[dr][j], TT2[dr][j], ps_o2[dr]
                    ps_kv, kvkey = kvb
                    vT = TT[:, 512:1024]
                    ch = 4 * i + cc
                    csl = slice(ch * 32, (ch + 1) * 32)
                    P.op('pe', lambda e: e.matmul(out=ps_o[:, cc * 32:(cc + 1) * 32], lhsT=vT[:, cc * 128:(cc + 1) * 128], rhs=scm[:, cc * 32:(cc + 1) * 32], start=True, stop=False),
                         [('TT', dr, j), ('scm', dr, j)], [('ps_o', dr)])
                    P.op('pe', lambda e: e.matmul(out=ps_o[:, cc * 32:(cc + 1) * 32], lhsT=Sb[:], rhs=A[:, csl], start=False, stop=True),
                         [('Sb', dr), ('A', dr)], [('ps_o', dr)])
                    P.op('dve', lambda e: e.scalar_tensor_tensor(out=Sf[:], in0=Sf[:], scalar=dec[:, ch:ch + 1], in1=ps_kv[:, 0:128], op0=ALU.mult, op1=ALU.add),
                         [kvkey, ('dec', dr), ('Sf', dr)], [('Sf', dr)])
                    chn = ch + 1 if fwd else ch - 1
                    if 0 <= chn < NCH:
                        P.op('act', lambda e: e.activation(out=Sb[:], in_=Sf[:], func=AF.Copy, scale=eref[:, chn:chn + 1]), [('Sf', dr), ('eref', dr)], [('Sb', dr)])

                def tile_tail(dr, i):
                    tsl = slice(i * 128, (i + 1) * 128)
                    P.op('dve', lambda e: e.tensor_add(out=oT[:, tsl], in0=oT[:, tsl], in1=ps_o2[dr][:, 0:128]), [('ps_o', dr), ('oT', i)], [('oT', i)])

                project([0, 4])
                project([1, 2])
                gate_bulk(0)
                prep(0)
                project([3])
                gate_bulk(1)
                prep(1)
                P.op('dve', lambda e: e.memset(oT[:], 0.0), [], [('oT', i) for i in range(NT)])
                for n_i in range(NT):
                    tiles = (n_i, NT - 1 - n_i)
                    for dr in range(2):
                        tile_head(dr, n_i, tiles[dr])
                    order = ((0, 1, 2, 3), (3, 2, 1, 0))
                    kvb = [emit_kv(dr, n_i, order[dr][0]) for dr in range(2)]
                    for step in range(4):
                        for dr in range(2):
                            nxt = emit_kv(dr, n_i, order[dr][step + 1]) if step < 3 else None
                            chunk(dr, n_i, tiles[dr], order[dr][step], kvb[dr])
                            kvb[dr] = nxt
                    for dr in range(2):
                        tile_tail(dr, tiles[dr])
                okeys = [('oT', i) for i in range(NT)]
                P.op('act', lambda e: e.activation(out=Eb[:], in_=oT[:], func=AF.Square), okeys, ['E'])
                for i in range(NT):
                    P.op('pe', lambda e, i=i: e.matmul(out=ps_sc1[:, 256 + i:257 + i], lhsT=Eb[:, i * 128:(i + 1) * 128], rhs=ones_f[:, 0:1], start=True, stop=True),
                         ['E', 'ones_f'], ['ps_sc'])
                P.op('dve', lambda e: e.tensor_copy(out=ssq_all[:, h, :], in_=ps_sc1[:, 256:256 + NT]), ['ps_sc'], ['ssq_all'])
                gn = SP[:, off_hn + l * 4 + h:off_hn + l * 4 + h + 1]
                P.op('dve', lambda e: e.scalar_tensor_tensor(out=A2[0][:], in0=oT[:], scalar=gn, in1=sg[:], op0=ALU.mult, op1=ALU.mult), okeys + ['sg', 'SP'], [('A', 0)])
                P.dma('sp', lambda e: e.dma_start(out=mixT_d[h, :, :], in_=A2[0][:]), [('A', 0)], [('mixT_d', h)], 'hA')
                P.barrier()

        for c in range(4):
            with ExitStack() as ph:
                zx = sb("lzx", [128, S + 4], F32, ph)
                xc = sb("lxc", [128, S], F32, ph)
                xcb = sb("lxcb", [128, S], BF16, ph)
                gy = sb("lgy", [128, S], BF16, ph)
                ua2 = [sb("lua%d" % r, [128, S], F32, ph) for r in range(2)]
                uu2 = [sb("luu%d" % r, [128, S], F32, ph) for r in range(2)]
                hf = sb("lhf", [128, S], F32, ph)
                hb = sb("lhb", [128, S], F32, ph)
                wz = sb("lwz", [128, 8, 256], BF16, ph)
                wbd = sb("lwbd", [128, 4, 128], BF16, ph)
                xb = [sb("lxb%d" % j, [128, 8, 512], BF16, ph) for j in range(2)]
                t1 = [sb("lt1%d" % j, [128, 512], F32, ph) for j in range(2)]
                t2 = [sb("lt2%d" % j, [128, 512], F32, ph) for j in range(2)]
                pz = [ps("lpz%d" % j, [128, 512], F32, ph) for j in range(4)]
                ps_ss = ps("lpss", [128, 512], F32, ph)

                for z in range(2):
                    colz = 2560 + z * 512 + c * 128
                    P.dma('pool', lambda e, z=z, colz=colz: e.dma_start(
                        out=wz[:, :, z * 128:(z + 1) * 128],
                        in_=w_in[l * D:(l + 1) * D, colz:colz + 128].rearrange("(k p) c -> p k c", p=128)), [], ['wz'], 'lwz')
                P.op('dve', lambda e: e.memset(wbd[:], 0.0), [], ['wbd'])
                for r in range(2):
                    for wi, wsrc in enumerate((lru_wa, lru_wx)):
                        for hh in range(2):
                            row0 = ((l * 2 + r) * 8 + 2 * c + hh) * 64
                            P.dma('pool', lambda e, r=r, wi=wi, hh=hh, row0=row0, wsrc=wsrc: e.dma_start(
                                out=wbd[hh * 64:(hh + 1) * 64, r * 2 + wi, hh * 64:(hh + 1) * 64], in_=wsrc[row0:row0 + 64, :]), [], ['wbd'], 'lwbd')
                P.op('dve', lambda e: e.memset(zx[:, 0:2], 0.0), [], ['zx'])
                P.op('dve', lambda e: e.memset(zx[:, S + 2:S + 4], 0.0), [], ['zx'])
                for tb in range(NTB):
                    j = tb % 2
                    tsl = slice(tb * 512, (tb + 1) * 512)
                    P.dma('sp', lambda e, tb=tb, j=j: e.dma_start(out=xb[j][:], in_=xT_d[:, :, tb * 512:(tb + 1) * 512].rearrange("k p t -> p k t")),
                          [('xT_d', tb)], [('xb', j)], 'lxb%d' % j)
                    for z in range(2):
                        pj = z + 2 * j
                        for k in range(8):
                            P.op('pe', lambda e, k=k, z=z, j=j, pj=pj: e.matmul(out=pz[pj][:, :], lhsT=wz[:, k, z * 128:(z + 1) * 128], rhs=xb[j][:, k, :], start=(k == 0), stop=(k == 7)),
                                 ['wz', ('xb', j)], [('pz', pj)])
                    P.op('act', lambda e, j=j, tb=tb: e.activation(out=zx[:, 2 + tb * 512:2 + (tb + 1) * 512], in_=pz[2 * j][:, :], func=AF.Copy), [('pz', 2 * j)], ['zx'])
                    pj = 1 + 2 * j
                    P.op('act', lambda e, j=j, pj=pj: e.activation(out=t1[j][:], in_=pz[pj][:, :], func=AF.Square), [('pz', pj)], [('t1', j)])
                    P.op('dve', lambda e, j=j: e.tensor_scalar(out=t1[j][:], in0=t1[j][:], scalar1=0.044715 * 1.5957691216, scalar2=1.5957691216, op0=ALU.mult, op1=ALU.add), [('t1', j)], [('t1', j)])
                    P.op('dve', lambda e, j=j, pj=pj: e.tensor_mul(out=t1[j][:], in0=t1[j][:], in1=pz[pj][:, :]), [('t1', j), ('pz', pj)], [('t1', j)])
                    P.op('act', lambda e, j=j: e.activation(out=t1[j][:], in_=t1[j][:], func=AF.Sigmoid), [('t1', j)], [('t1', j)])
                    P.op('dve', lambda e, j=j, pj=pj, tsl=tsl: e.tensor_mul(out=gy[:, tsl], in0=t1[j][:], in1=pz[pj][:, :]), [('t1', j), ('pz', pj)], ['gy'])
                cw = lambda jj: SP[:, off_cw + (l * 4 + jj) * 4 + c:off_cw + (l * 4 + jj) * 4 + c + 1]
                cb = SP[:, off_cb + l * 4 + c:off_cb + l * 4 + c + 1]
                P.op('dve', lambda e: e.tensor_scalar(out=xc[:], in0=zx[:, 0:S], scalar1=cw(0), scalar2=cb, op0=ALU.mult, op1=ALU.add), ['zx', 'SP'], ['xc'])
                for jj in range(1, 4):
                    P.op('dve', lambda e, jj=jj: e.scalar_tensor_tensor(out=xc[:], in0=zx[:, jj:jj + S], scalar=cw(jj), in1=xc[:], op0=ALU.mult, op1=ALU.add), ['zx', 'SP', 'xc'], ['xc'])
                P.op('act', lambda e: e.activation(out=xcb[:], in_=xc[:], func=AF.Copy), ['xc'], ['xcb'])
                cols2 = [l * 8 + r * 4 + c for r in range(2)]
                hd2 = [hf, hb]
                hk2 = ['hf', 'hb']
                for tb in range(NTB):
                    tsl = slice(tb * 512, (tb + 1) * 512)
                    for r in range(2):
                        col = cols2[r]
                        ba = SP[:, off_ba + col:off_ba + col + 1]
                        bx = SP[:, off_bx + col:off_bx + col + 1]
                        P.op('pe', lambda e, r=r, tsl=tsl: e.matmul(out=pz[2 * r][:, :], lhsT=wbd[:, r * 2, :], rhs=xcb[:, tsl], start=True, stop=True), ['wbd', 'xcb'], [('pz', 2 * r)])
                        P.op('pe', lambda e, r=r, tsl=tsl: e.matmul(out=pz[2 * r + 1][:, :], lhsT=wbd[:, r * 2 + 1, :], rhs=xcb[:, tsl], start=True, stop=True), ['wbd', 'xcb'], [('pz', 2 * r + 1)])
                        P.op('act', lambda e, r=r, tsl=tsl, ba=ba: e.activation(out=ua2[r][:, tsl], in_=pz[2 * r][:, :], func=AF.Sigmoid, bias=ba), [('pz', 2 * r), 'SP'], [('ua', r)])
                        P.op('act', lambda e, r=r, tsl=tsl, bx=bx: e.activation(out=uu2[r][:, tsl], in_=pz[2 * r + 1][:, :], func=AF.Sigmoid, bias=bx), [('pz', 2 * r + 1), 'SP'], [('uu', r)])
                for r in range(2):
                    P.op('act', lambda e, r=r: e.activation(out=hd2[r][:], in_=ua2[r][:], func=AF.Exp, scale=CP2[:, cols2[r]:cols2[r] + 1]), [('ua', r), 'CP2'], [hk2[r]])
                for r in range(2):
                    P.op('act', lambda e, r=r: e.activation(out=ua2[r][:], in_=ua2[r][:], func=AF.Exp, scale=CP[:, cols2[r]:cols2[r] + 1]), [('ua', r), 'CP', hk2[r]], [('ua', r)])
                for r in range(2):
                    P.op('dve', lambda e, r=r: e.tensor_scalar(out=hd2[r][:], in0=hd2[r][:], scalar1=-1.0, scalar2=1.0, op0=ALU.mult, op1=ALU.add), [hk2[r]], [hk2[r]])
                    P.op('dve', lambda e, r=r: e.tensor_scalar(out=hd2[r][:], in0=hd2[r][:], scalar1=0.0, scalar2=None, op0=ALU.max), [hk2[r]], [hk2[r]])
                    P.op('dve', lambda e, r=r: e.tensor_mul(out=uu2[r][:], in0=uu2[r][:], in1=xc[:]), [('uu', r), 'xc'], [('uu', r)])
                for r in range(2):
                    P.op('act', lambda e, r=r: e.activation(out=hd2[r][:], in_=hd2[r][:], func=AF.Sqrt), [hk2[r]], [hk2[r]])
                for r in range(2):
                    P.op('dve', lambda e, r=r: e.tensor_mul(out=uu2[r][:], in0=uu2[r][:], in1=hd2[r][:]), [('uu', r), hk2[r]], [('uu', r)])
                    V = (lambda ap: ap) if r == 0 else (lambda ap: ap[:, ::-1])
                    P.op('dve', lambda e, V=V, r=r: e.tensor_tensor_scan(out=V(hd2[r][:]), data0=V(ua2[r][:]), data1=V(uu2[r][:]), initial=0.0, op0=ALU.mult, op1=ALU.add),
                         [('ua', r), ('uu', r), hk2[r]], [hk2[r]])
                P.op('dve', lambda e: e.tensor_add(out=hf[:], in0=hf[:], in1=hb[:]), ['hf', 'hb'], ['hf'])
                P.op('act', lambda e: e.activation(out=hb[:], in_=hf[:], func=AF.Square), ['hf', 'hb'], ['hb'])
                for i in range(NT):
                    P.op('pe', lambda e, i=i: e.matmul(out=ps_ss[:, i:i + 1], lhsT=hb[:, i * 128:(i + 1) * 128], rhs=ones_f[:, 0:1], start=True, stop=True), ['hb', 'ones_f'], ['ps_ss'])
                P.op('dve', lambda e: e.tensor_copy(out=ssq_all[:, 4 + c, :], in_=ps_ss[:, 0:NT]), ['ps_ss'], ['ssq_all'])
                gn = SP[:, off_ln + l * 4 + c:off_ln + l * 4 + c + 1]
                P.op('dve', lambda e: e.scalar_tensor_tensor(out=xcb[:], in0=hf[:], scalar=gn, in1=gy[:], op0=ALU.mult, op1=ALU.mult), ['hf', 'gy', 'SP'], ['xcb'])
                dump("xcbf", xcb[:], [128, S], BF16, ['xcb'])
                dump("hff", hf[:], [128, S], F32, ['hf'])
                P.dma('sp', lambda e: e.dma_start(out=mixT_d[4 + c, :, :], in_=xcb[:]), ['xcb'], [('mixT_d', 4 + c)], 'lxcb')
                P.barrier()

        with ExitStack() as pm:
            lg_all = sb("wlg", [128, NT, NE], F32, pm)
            bidx16 = sb("rbidx16", [128, NB], I32, pm)
            wk = sb("rwk", [128, NT, 4], F32, pm)
            desti = sb("rdesti", [128, NT, 4], I32, pm)
            widx = sb("rwidx", [128, 3, NB, 8], I32, pm)
            bidx = sb("rbidx", [128, NB], I32, pm)
            st6 = sb("wst6", [128, 2, 6], F32, pm)
            mv = sb("wmv", [128, 2], F32, pm)
            rstd = sb("wrstd", [128, 1], F32, pm)
            def load_lnr(st, r0):
                t = sb("wlnr", [128, 2, D], F32, st)
                for r4 in range(2):
                    P.dma('sp', lambda e, r4=r4: e.dma_start(out=t[:, r4, :], in_=lnp[l * 4 + r0 + r4:l * 4 + r0 + r4 + 1, :].to_broadcast([128, D])), [], ['lnr'], 'wlnr')
                return t

            def layernorm(z, zkey, lnr, dst, dstkey):
                gi = 0
                zz = z[:].rearrange("p (a f) -> p a f", a=2)
                for a in range(2):
                    P.op('dve', lambda e, a=a: e.bn_stats(out=st6[:, a, :], in_=zz[:, a, :]), [zkey], ['st6'])
                P.op('dve', lambda e: e.bn_aggr(out=mv[:], in_=st6[:]), ['st6'], ['mv'])
                P.op('dve', lambda e: e.tensor_scalar(out=rstd[:], in0=mv[:, 1:2], scalar1=LN_EPS, scalar2=None, op0=ALU.add), ['mv'], ['rstd'])
                P.op('act', lambda e: e.activation(out=rstd[:], in_=rstd[:], func=AF.Sqrt), ['rstd'], ['rstd'])
                P.op('dve', lambda e: e.reciprocal(out=rstd[:], in_=rstd[:]), ['rstd'], ['rstd'])
                P.op('dve', lambda e: e.tensor_scalar(out=z[:], in0=z[:], scalar1=mv[:, 0:1], scalar2=rstd[:, 0:1], op0=ALU.subtract, op1=ALU.mult), [zkey, 'mv', 'rstd'], [zkey])
                P.op('dve', lambda e: e.tensor_mul(out=z[:], in0=z[:], in1=lnr[:, gi, :]), [zkey, 'lnr'], [zkey])
                P.op('dve', lambda e: e.tensor_add(out=dst[:], in0=z[:], in1=lnr[:, gi + 1, :]), [zkey, 'lnr'], [dstkey])

            with ExitStack() as ph:
                lnr1 = load_lnr(ph, 0)
                wo = sb("wwo", [128, 8, D], BF16, ph)
                rw = sb("wrw", [128, 8, NE], F32, ph)
                rb = sb("wrb", [128, NE], F32, ph)
                mt = [sb("wmt%d" % j, [128, 8, 128], BF16, ph) for j in range(2)]
                xt = [sb("wxt%d" % j, [128, D], F32, ph) for j in range(2)]
                acc = [sb("wacc%d" % j, [128, D], F32, ph) for j in range(2)]
                x1 = [sb("wx1%d" % j, [128, D], F32, ph) for j in range(2)]
                x1b = [sb("wx1b%d" % j, [128, D], BF16, ph) for j in range(2)]
                x1T = [sb("wx1T%d" % j, [128, D], F32, ph) for j in range(2)]
                py = [ps("wpy%d" % j, [128, 512], F32, ph) for j in range(5)]
                pT = ps("wpT", [128, 1024], F32, ph)
                plg = ps("wplg", [128, 512], F32, ph)

                P.dma('pool', lambda e: e.dma_start(out=wo[:], in_=w_out[l * D:(l + 1) * D, :].rearrange("(k p) c -> p k c", p=128)), [], ['wo'], 'wwo')
                P.dma('sp', lambda e: e.dma_start(out=rw[:], in_=router_w[l * D:(l + 1) * D, :].rearrange("(k p) c -> p k c", p=128)), [], ['rw'], 'wrw')
                P.dma('sp', lambda e: e.dma_start(out=rb[:], in_=router_b[l:l + 1, :].to_broadcast([128, NE])), [], ['rb'], 'wrb')
                P.op('dve', lambda e: e.tensor_scalar(out=rs_all[:, 0:4, :], in0=ssq_all[:, 0:4, :], scalar1=1.0 / 128, scalar2=RMS_EPS, op0=ALU.mult, op1=ALU.add), ['ssq_all'], ['rs_all'])
                P.op('dve', lambda e: e.tensor_add(out=rs_all[:, 4, :], in0=ssq_all[:, 4, :], in1=ssq_all[:, 5, :]), ['ssq_all', 'rs_all'], ['rs_all'])
                P.op('dve', lambda e: e.tensor_add(out=rs_all[:, 4, :], in0=rs_all[:, 4, :], in1=ssq_all[:, 6, :]), ['ssq_all', 'rs_all'], ['rs_all'])
                P.op('dve', lambda e: e.tensor_add(out=rs_all[:, 4, :], in0=rs_all[:, 4, :], in1=ssq_all[:, 7, :]), ['ssq_all', 'rs_all'], ['rs_all'])
                P.op('dve', lambda e: e.tensor_scalar(out=rs_all[:, 4, :], in0=rs_all[:, 4, :], scalar1=1.0 / 512, scalar2=RMS_EPS, op0=ALU.mult, op1=ALU.add), ['rs_all'], ['rs_all'])
                P.op('act', lambda e: e.activation(out=rs_all[:], in_=rs_all[:], func=AF.Sqrt), ['rs_all'], ['rs_all'])
                P.op('dve', lambda e: e.reciprocal(out=rs_all[:], in_=rs_all[:]), ['rs_all'], ['rs_all'])

                for i in range(NT):
                    j = i % 2
                    rsl = slice(i * 128, (i + 1) * 128)
                    P.dma('sp', lambda e, i=i, j=j: e.dma_start(out=mt[j][:], in_=mixT_d[:, :, i * 128:(i + 1) * 128].rearrange("k p t -> p k t")),
                          [], [('mt', j)], 'wmt%d' % j)
                    P.dma('sp', lambda e, j=j, rsl=rsl: e.dma_start(out=xt[j][:], in_=xcur[rsl, :]), [], [('xt', j)], 'wxt%d' % j)
                    for dh in range(2):
                        dsl = slice(dh * 512, (dh + 1) * 512)
                        for gq in range(5):
                            ks = [gq] if gq < 4 else [4, 5, 6, 7]
                            for ki, k in enumerate(ks):
                                P.op('pe', lambda e, k=k, gq=gq, j=j, dsl=dsl, ki=ki, n=len(ks): e.matmul(out=py[gq][:, :], lhsT=mt[j][:, k, :], rhs=wo[:, k, dsl], start=(ki == 0), stop=(ki == n - 1)),
                                     ['wo', ('mt', j)], [('py', gq)])
                        P.op('dve', lambda e, j=j, dsl=dsl, i=i: e.tensor_scalar(out=acc[j][:, dsl], in0=py[0][:, :], scalar1=rs_all[:, 0, i:i + 1], scalar2=None, op0=ALU.mult), [('py', 0), 'rs_all'], [('acc', j)])
                        for gq in range(1, 5):
                            P.op('dve', lambda e, j=j, dsl=dsl, i=i, gq=gq: e.scalar_tensor_tensor(out=acc[j][:, dsl], in0=py[gq][:, :], scalar=rs_all[:, gq, i:i + 1], in1=acc[j][:, dsl], op0=ALU.mult, op1=ALU.add),
                                 [('py', gq), 'rs_all', ('acc', j)], [('acc', j)])
                    P.op('dve', lambda e, j=j: e.scalar_tensor_tensor(out=acc[j][:], in0=xt[j][:], scalar=DN_ALPHA, in1=acc[j][:], op0=ALU.mult, op1=ALU.add), [('xt', j), ('acc', j)], [('acc', j)])
                    layernorm(acc[j], ('acc', j), lnr1, x1[j], ('x1', j))
                    P.dma('sp', lambda e, j=j, rsl=rsl: e.dma_start(out=x1_d[rsl, :], in_=x1[j][:]), [('x1', j)], [('x1_d', i)], 'wx1%d' % j)
                    P.op('act', lambda e, j=j: e.activation(out=x1b[j][:], in_=x1[j][:], func=AF.Copy), [('x1', j)], [('x1b', j)])
                    P.dma('sp', lambda e, j=j, rsl=rsl: e.dma_start(out=x1b_d[rsl, :], in_=x1b[j][:]), [('x1b', j)], [('x1b_d', i)], 'wx1b%d' % j)
                    for k in range(8):
                        P.op('pe', lambda e, k=k, j=j: e.transpose(out=pT[:, k * 128:(k + 1) * 128], in_=x1[j][:, k * 128:(k + 1) * 128], identity=ident), [('x1', j), 'cst'], ['pT'])
                    P.op('act', lambda e, j=j: e.activation(out=x1T[j][:], in_=pT[:, :], func=AF.Copy), ['pT'], [('x1T', j)])
                    for k in range(8):
                        P.op('pe', lambda e, k=k, j=j: e.matmul(out=plg[:, 0:NE], lhsT=x1T[j][:, k * 128:(k + 1) * 128], rhs=rw[:, k, :], start=(k == 0), stop=(k == 7)), [('x1T', j), 'rw'], ['plg'])
                    P.op('dve', lambda e, i=i: e.tensor_add(out=lg_all[:, i, :], in0=plg[:, 0:NE], in1=rb[:]), ['plg', 'rb'], ['lg_all'])
                P.barrier()

            with ExitStack() as ph:
                m8 = sb("rm8", [128, NT, 8], F32, ph)
                wsum = sb("rwsum", [128, NT], F32, ph)
                M = sb("rM", [128, NT, NE], BF16, ph)
                Msum = sb("rMsum", [128, NE], F32, ph)
                Msb = sb("rMsb", [128, NE], BF16, ph)
                rank = sb("rrank", [128, NT, NE], F32, ph)
                tmp3 = sb("rtmp3", [128, NT, NE], F32, ph)
                ustrb = sb("rustrb", [128, 128], BF16, ph)
                cnt = sb("rcnt", [128, NE], F32, ph)
                pad = sb("rpad", [128, NE], F32, ph)
                pend = sb("rpend", [128, NE], F32, ph)
                pstart = sb("rpst", [128, NE], F32, ph)
                destf = sb("rdestf", [128, NT, 4], F32, ph)
                cmp3 = sb("rcmp3", [128, NB, NE], F32, ph)
                ebf = sb("rebf", [128, NB], F32, ph)
                basef = sb("rbasef", [128, NB], F32, ph)
                tmpb = sb("rtmpb", [128, NB], F32, ph)
                xs = [sb("rxs%d" % j, [128, D], BF16, ph) for j in range(2)]
                prk = [ps("rprk%d" % j, [128, 512], F32, ph) for j in range(2)]

                P.op('dve', lambda e: e.tensor_copy(out=ustrb[:], in_=cst[:, 481:609]), ['cst'], ['ustrb'])
                for i in range(NT):
                    P.op('dve', lambda e, i=i: e.max(out=m8[:, i, :], in_=lg_all[:, i, :]), ['lg_all'], ['m8'])
                P.op('dve', lambda e: e.tensor_tensor(out=M[:], in0=lg_all[:], in1=m8[:, :, 3:4].to_broadcast([128, NT, NE]), op=ALU.is_ge), ['lg_all', 'm8'], ['M'])
                P.op('dve', lambda e: e.tensor_sub(out=wk[:], in0=m8[:, :, 0:4], in1=m8[:, :, 0:1].to_broadcast([128, NT, 4])), ['m8'], ['wk'])
                P.op('act', lambda e: e.activation(out=wk[:], in_=wk[:], func=AF.Exp), ['wk'], ['wk'])
                P.op('dve', lambda e: e.reduce_sum(out=wsum[:], in_=wk[:], axis=AX.X), ['wk'], ['wsum'])
                P.op('dve', lambda e: e.reciprocal(out=wsum[:], in_=wsum[:]), ['wsum'], ['wsum'])
                P.op('dve', lambda e: e.tensor_mul(out=wk[:], in0=wk[:], in1=wsum[:].unsqueeze(2).to_broadcast([128, NT, 4])), ['wk', 'wsum'], ['wk'])
                P.op('dve', lambda e: e.memset(Msum[:], 0.0), [], ['Msum'])
                for i in range(NT):
                    pj = i % 2
                    P.op('pe', lambda e, i=i, pj=pj: e.matmul(out=prk[pj][:, 0:NE], lhsT=ustrb[:], rhs=M[:, i, :], start=True, stop=(i == 0)), ['ustrb', 'M'], [('prk', pj)])
                    if i > 0:
                        P.op('pe', lambda e, pj=pj: e.matmul(out=prk[pj][:, 0:NE], lhsT=ones_b[:, 0:128], rhs=Msb[:], start=False, stop=True), ['ones_b', 'Msb'], [('prk', pj)])
                    P.op('dve', lambda e, i=i, pj=pj: e.tensor_copy(out=rank[:, i, :], in_=prk[pj][:, 0:NE]), [('prk', pj)], ['rank'])
                    P.op('dve', lambda e, i=i: e.tensor_add(out=Msum[:], in0=Msum[:], in1=M[:, i, :]), ['Msum', 'M'], ['Msum'])
                    P.op('dve', lambda e: e.tensor_copy(out=Msb[:], in_=Msum[:]), ['Msum'], ['Msb'])
                P.op('pe', lambda e: e.matmul(out=prk[0][:, 64:64 + NE], lhsT=ones_b[:, 0:128], rhs=Msb[:], start=True, stop=True), ['ones_b', 'Msb'], [('prk', 0)])
                P.op('dve', lambda e: e.tensor_copy(out=cnt[:], in_=prk[0][:, 64:64 + NE]), [('prk', 0)], ['cnt'])
                P.op('dve', lambda e: e.memset(pad[:], 0.0), [], ['pad'])
                for m in range(NT // 4 + 1):
                    P.op('dve', lambda e, m=m: e.scalar_tensor_tensor(out=pad[:], in0=cnt[:], scalar=512.0 * m, in1=pad[:], op0=ALU.is_gt, op1=ALU.add), ['cnt', 'pad'], ['pad'])
                P.op('dve', lambda e: e.tensor_scalar(out=pad[:], in0=pad[:], scalar1=512.0, scalar2=None, op0=ALU.mult), ['pad'], ['pad'])
                P.op('dve', lambda e: e.tensor_tensor_scan(out=pend[:], data0=ones_f[:, 0:NE], data1=pad[:], initial=0.0, op0=ALU.mult, op1=ALU.add), ['pad', 'ones_f'], ['pend'])
                P.op('dve', lambda e: e.tensor_sub(out=pstart[:], in0=pend[:], in1=pad[:]), ['pend', 'pad'], ['pstart'])
                P.op('dve', lambda e: e.tensor_add(out=rank[:], in0=rank[:], in1=pstart[:].unsqueeze(1).to_broadcast([128, NT, NE])), ['rank', 'pstart'], ['rank'])
                for k in range(4):
                    P.op('dve', lambda e, k=k: e.tensor_tensor(out=tmp3[:], in0=lg_all[:], in1=m8[:, :, k:k + 1].to_broadcast([128, NT, NE]), op=ALU.is_equal), ['lg_all', 'm8'], ['tmp3'])
                    P.op('dve', lambda e: e.tensor_mul(out=tmp3[:], in0=tmp3[:], in1=rank[:]), ['tmp3', 'rank'], ['tmp3'])
                    P.op('dve', lambda e, k=k: e.reduce_sum(out=destf[:, :, k], in_=tmp3[:], axis=AX.X), ['tmp3'], ['destf'])
                P.op('dve', lambda e: e.tensor_copy(out=desti[:], in_=destf[:]), ['destf'], ['desti'])
                P.op('dve', lambda e: e.tensor_tensor(out=cmp3[:], in0=pend[:].unsqueeze(1).to_broadcast([128, NB, NE]), in1=thr[:, 0:NB].unsqueeze(2).to_broadcast([128, NB, NE]), op=ALU.is_le), ['pend', 'cst'], ['cmp3'])
                P.op('dve', lambda e: e.reduce_sum(out=ebf[:], in_=cmp3[:], axis=AX.X), ['cmp3'], ['ebf'])
                P.op('dve', lambda e: e.tensor_scalar(out=ebf[:], in0=ebf[:], scalar1=float(NE - 1), scalar2=float(l * NE), op0=ALU.min, op1=ALU.add), ['ebf'], ['ebf'])
                P.op('dve', lambda e: e.tensor_copy(out=bidx[:], in_=ebf[:]), ['ebf'], ['bidx'])
                P.op('dve', lambda e: e.tensor_scalar(out=tmpb[:], in0=ebf[:], scalar1=16.0, scalar2=cst[:, 609:610], op0=ALU.mult, op1=ALU.add), ['ebf', 'cst'], ['tmpb'])
                P.op('dve', lambda e: e.tensor_copy(out=bidx16[:], in_=tmpb[:]), ['tmpb'], ['bidx16'])
                P.op('dve', lambda e: e.tensor_scalar(out=basef[:], in0=ebf[:], scalar1=1024.0, scalar2=iota_p, op0=ALU.mult, op1=ALU.add), ['ebf', 'cst'], ['basef'])
                for k in range(8):
                    P.op('dve', lambda e, k=k: e.tensor_scalar(out=widx[:, 2, :, k], in0=basef[:], scalar1=float(k * 128), scalar2=None, op0=ALU.add), ['basef'], ['widx'])
                    P.op('dve', lambda e, k=k: e.tensor_scalar(out=tmpb[:], in0=basef[:], scalar1=float(k * 128), scalar2=2.0, op0=ALU.add, op1=ALU.mult), ['basef'], ['tmpb'])
                    P.op('dve', lambda e, k=k: e.tensor_copy(out=widx[:, 0, :, k], in_=tmpb[:]), ['tmpb'], ['widx'])
                    P.op('dve', lambda e, k=k: e.tensor_scalar(out=widx[:, 1, :, k], in0=tmpb[:], scalar1=1.0, scalar2=None, op0=ALU.add), ['tmpb'], ['widx'])
                for i in range(NT):
                    j = i % 2
                    P.dma('sp', lambda e, i=i, j=j: e.dma_start(out=xs[j][:], in_=x1b_d[i * 128:(i + 1) * 128, :]), [], [('xs', j)], 'rxsl%d' % j)
                    for k in range(4):
                        P.dma('pool', lambda e, i=i, j=j, k=k: e.indirect_dma_start(out=xg_d, out_offset=bass.IndirectOffsetOnAxis(ap=desti[:, i, k:k + 1], axis=0), in_=xs[j][:], in_offset=None),
                              [('xs', j), 'desti'], [('xg', i, k)], 'rxs%d' % j)
                P.barrier()

            with ExitStack() as ph:
                wsl = [sb("fw%d" % j, [128, 8, D], BF16, ph) for j in range(6)]
                bg16 = [sb("fbg%d" % j, [128, 128], F32, ph) for j in range(2)]
                bgT = [sb("fbgT%d" % j, [128, 16], F32, ph) for j in range(2)]
                bdn = [sb("fbd%d" % j, [128, D], F32, ph) for j in range(2)]
                xgt = [sb("fxg%d" % j, [128, 4, D], BF16, ph) for j in range(2)]
                xgT = [sb("fxgT%d" % j, [128, 8, 512], BF16, ph) for j in range(2)]
                hid = sb("fhid", [128, 8, 512], BF16, ph)
                tg = [sb("ftg%d" % j, [128, 512], F32, ph) for j in range(2)]
                ts = [sb("fts%d" % j, [128, 512], F32, ph) for j in range(2)]
                tu = [sb("ftu%d" % j, [128, 512], F32, ph) for j in range(2)]
                yt = [sb("fyt%d" % j, [128, D], F32, ph) for j in range(2)]
                pxt = [ps("fpx%d" % j, [128, 1024], BF16, ph) for j in range(2)]
                pg = [ps("fpg%d" % j, [128, 512], F32, ph) for j in range(2)]
                pu = [ps("fpu%d" % j, [128, 512], F32, ph) for j in range(2)]
                pd = [ps("fpd%d" % j, [128, 512], F32, ph) for j in range(2)]
                wgu2 = w_gu.rearrange("r (h c) -> (r h) c", h=2)
                wsrc = [wgu2, wgu2, w_dn]
                bgu16 = b_gu.rearrange("r (c q) -> (r c) q", q=128)
                nyt = [0]

                def f_load(b):
                    j = b % 2
                    for q in range(3):
                        sl = (3 * b + q) % 6
                        for k in range(8):
                            P.dma('pool', lambda e, q=q, k=k, sl=sl: e.indirect_dma_start(out=wsl[sl][:, k, :], out_offset=None, in_=wsrc[q],
                                  in_offset=bass.IndirectOffsetOnAxis(ap=widx[:, q, b, k:k + 1], axis=0)), ['widx'], [('wsl', sl)], 'fw%d' % sl)
                    P.dma('pool', lambda e: e.indirect_dma_start(out=bg16[j][:], out_offset=None, in_=bgu16, in_offset=bass.IndirectOffsetOnAxis(ap=bidx16[:, b:b + 1], axis=0)), ['bidx16'], [('bg16', j)], 'fbg%d' % j)
                    P.dma('pool', lambda e: e.indirect_dma_start(out=bdn[j][:], out_offset=None, in_=b_dn, in_offset=bass.IndirectOffsetOnAxis(ap=bidx[:, b:b + 1], axis=0)), ['bidx'], [('bdn', j)], 'fbd%d' % j)
                    P.dma('sp', lambda e: e.dma_start(out=xgt[j][:], in_=xg_d[b * 512:(b + 1) * 512, :].rearrange("(r p) d -> p r d", p=128)), [], [('xgt', j)], 'fxg%d' % j)

                def f_transposes(b):
                    j = b % 2
                    for dk in range(8):
                        pj = dk % 2
                        for r in range(4):
                            P.op('pe', lambda e, dk=dk, r=r, pj=pj: e.transpose(out=pxt[pj][:, r * 128:(r + 1) * 128], in_=xgt[j][:, r, dk * 128:(dk + 1) * 128], identity=identb[:]),
                                 [('xgt', j), 'identb'], [('pxt', pj)])
                        P.op('act', lambda e, dk=dk, pj=pj: e.activation(out=xgT[j][:, dk, :], in_=pxt[pj][:, 0:512], func=AF.Copy), [('pxt', pj)], [('xgT', j)])
                    P.op('pe', lambda e: e.transpose(out=pd[0][:, 0:128], in_=bg16[j][:, :], identity=ident), [('bg16', j), 'cst'], [('pd', 0)])
                    P.op('act', lambda e: e.activation(out=bgT[j][:], in_=pd[0][:, 0:16], func=AF.Copy), [('pd', 0)], [('bgT', j)])

                def f_gateup(b):
                    j = b % 2
                    sG, sU = (3 * b) % 6, (3 * b + 1) % 6
                    for jc in range(8):
                        pj = jc % 2
                        for (sl, pp, ppk) in ((sG, pg[pj], ('pg', pj)), (sU, pu[pj], ('pu', pj))):
                            for k in range(8):
                                P.op('pe', lambda e, sl=sl, pp=pp, k=k, jc=jc: e.matmul(out=pp[:, :], lhsT=wsl[sl][:, k, jc * 128:(jc + 1) * 128], rhs=xgT[j][:, k, :], start=(k == 0), stop=(k == 7)),
                                     [('wsl', sl), ('xgT', j)], [ppk])
                        P.op('dve', lambda e, pj=pj, jc=jc: e.tensor_scalar(out=tg[pj][:], in0=pg[pj][:, :], scalar1=bgT[j][:, jc:jc + 1], scalar2=7.0, op0=ALU.add, op1=ALU.min), [('pg', pj), ('bgT', j)], [('tg', pj)])
                        P.op('act', lambda e, pj=pj: e.activation(out=ts[pj][:], in_=tg[pj][:], func=AF.Sigmoid, scale=1.702), [('tg', pj)], [('ts', pj)])
                        P.op('act', lambda e, pj=pj, jc=jc: e.activation(out=tu[pj][:], in_=pu[pj][:, :], func=AF.Identity, bias=bgT[j][:, 8 + jc:9 + jc]), [('pu', pj), ('bgT', j)], [('tu', pj)])
                        P.op('dve', lambda e, pj=pj: e.tensor_mul(out=tg[pj][:], in0=tg[pj][:], in1=ts[pj][:]), [('tg', pj), ('ts', pj)], [('tg', pj)])
                        P.op('dve', lambda e, pj=pj: e.tensor_scalar(out=tu[pj][:], in0=tu[pj][:], scalar1=7.0, scalar2=-7.0, op0=ALU.min, op1=ALU.max), [('tu', pj)], [('tu', pj)])
                        P.op('dve', lambda e, pj=pj, jc=jc: e.scalar_tensor_tensor(out=hid[:, jc, :], in0=tu[pj][:], scalar=1.0, in1=tg[pj][:], op0=ALU.add, op1=ALU.mult), [('tu', pj), ('tg', pj)], ['hid'])

                def f_down(b):
                    j = b % 2
                    sD = (3 * b + 2) % 6
                    for rt in range(4):
                        yj = nyt[0] % 2
                        nyt[0] += 1
                        for dh in range(2):
                            for jc in range(8):
                                P.op('pe', lambda e, rt=rt, dh=dh, jc=jc: e.matmul(out=pd[dh][:, :], lhsT=hid[:, jc, rt * 128:(rt + 1) * 128], rhs=wsl[sD][:, jc, dh * 512:(dh + 1) * 512], start=(jc == 0), stop=(jc == 7)),
                                     ['hid', ('wsl', sD)], [('pd', dh)])
                            P.op('dve', lambda e, dh=dh, yj=yj: e.tensor_add(out=yt[yj][:, dh * 512:(dh + 1) * 512], in0=pd[dh][:, :], in1=bdn[j][:, dh * 512:(dh + 1) * 512]), [('pd', dh), ('bdn', j)], [('yt', yj)])
                        P.dma('sp', lambda e, rt=rt, yj=yj: e.dma_start(out=yg_d[b * 512 + rt * 128:b * 512 + (rt + 1) * 128, :], in_=yt[yj][:]), [('yt', yj)], [('yg', b, rt)], 'fyt%d' % yj)

                f_load(0)
                f_transposes(0)
                for b in range(NB):
                    if b + 1 < NB:
                        f_load(b + 1)
                    f_gateup(b)
                    if b + 1 < NB:
                        f_transposes(b + 1)
                    f_down(b)
                P.barrier()

            with ExitStack() as ph:
                lnr2 = load_lnr(ph, 2)
                gk = [sb("cgk%d" % j, [128, 4, D], F32, ph) for j in range(2)]
                x1t = [sb("cx1%d" % j, [128, D], F32, ph) for j in range(2)]
                x2 = [sb("cx2%d" % j, [128, D], F32, ph) for j in range(2)]
                xTb = [sb("cxb%d" % j, [128, 8, 128], BF16, ph) for j in range(2)]
                pT2 = [ps("cpT%d" % j, [128, 1024], F32, ph) for j in range(2)]
                for i in range(NT):
                    j = i % 2
                    rsl = slice(i * 128, (i + 1) * 128)
                    for k in range(4):
                        P.dma('pool', lambda e, i=i, j=j, k=k: e.indirect_dma_start(out=gk[j][:, k, :], out_offset=None, in_=yg_d, in_offset=bass.IndirectOffsetOnAxis(ap=desti[:, i, k:k + 1], axis=0)),
                              ['desti'], [('gk', j)], 'cgk%d' % j)
                    P.dma('sp', lambda e, j=j, rsl=rsl: e.dma_start(out=x1t[j][:], in_=x1_d[rsl, :]), [], [('x1t', j)], 'cx1%d' % j)
                    P.op('dve', lambda e, j=j, i=i: e.scalar_tensor_tensor(out=x1t[j][:], in0=x1t[j][:], scalar=DN_ALPHA, in1=x1t[j][:], op0=ALU.mult, op1=ALU.bypass) if False else
                         e.tensor_scalar(out=x1t[j][:], in0=x1t[j][:], scalar1=DN_ALPHA, scalar2=None, op0=ALU.mult), [('x1t', j)], [('x1t', j)])
                    for k in range(4):
                        P.op('dve', lambda e, j=j, i=i, k=k: e.scalar_tensor_tensor(out=x1t[j][:], in0=gk[j][:, k, :], scalar=wk[:, i, k:k + 1], in1=x1t[j][:], op0=ALU.mult, op1=ALU.add),
                             [('gk', j), 'wk', ('x1t', j)], [('x1t', j)])
                    layernorm(x1t[j], ('x1t', j), lnr2, x2[j], ('x2', j))
                    P.dma('sp', lambda e, j=j, rsl=rsl: e.dma_start(out=xnext[rsl, :], in_=x2[j][:]), [('x2', j)], [('xres', i)], 'cx2%d' % j)
                    if l < L - 1:
                        emit_xT(ph, x2[j], ('x2', j), i, pT2[j], xTb[j], j)
                P.barrier()
    es.close()
    return nc


def make_consts():
    c = np.zeros((128, NCON), np.float32)
    c[:, 0:128] = np.eye(128, dtype=np.float32)
    s = np.arange(32)[:, None]
    t = np.arange(32)[None, :]
    c[0:32, 128:256] = np.tile((s <= t).astype(np.float32), (1, 4))
    c[0:32, 256:384] = np.tile((s >= t).astype(np.float32), (1, 4))
    c[:, 384] = np.arange(128)
    c[:, 385:449] = (np.arange(64) * 512)[None, :]
    c[:, 449:481] = np.arange(32)[None, :]
    c[:, 481:609] = (np.arange(128)[:, None] < np.arange(128)[None, :]).astype(np.float32)
    c[:, 609] = np.arange(128) % 16
    return c


def pack_inputs(inp, L):
    f = lambda a: np.ascontiguousarray(np.asarray(a, dtype=np.float32))
    smallp = np.concatenate([
        f(inp["hg_lb"]).reshape(L * 8, 128), f(inp["hg_norm"]).reshape(L * 4, 128),
        f(inp["lru_conv_w"]).reshape(L * 16, 128), f(inp["lru_conv_b"]).reshape(L * 4, 128),
        f(inp["lru_ba"]).reshape(L * 8, 128), f(inp["lru_bx"]).reshape(L * 8, 128),
        f(inp["lru_lambda"]).reshape(L * 8, 128), f(inp["lru_norm"]).reshape(L * 4, 128)], axis=0)
    lnp = np.stack([f(inp["ln1_g"]), f(inp["ln1_b"]), f(inp["ln2_g"]), f(inp["ln2_b"])], axis=1).reshape(L * 4, D)
    return {
        "w_in": f(inp["w_in"]).reshape(L * D, 3584),
        "smallp": np.ascontiguousarray(smallp),
        "lru_wa": f(inp["lru_wa"]).reshape(L * 2 * 8 * 64, 64),
        "lru_wx": f(inp["lru_wx"]).reshape(L * 2 * 8 * 64, 64),
        "w_out": f(inp["w_out"]).reshape(L * D, D),
        "lnp": np.ascontiguousarray(lnp),
        "router_w": f(inp["router_w"]).reshape(L * D, NE),
        "router_b": f(inp["router_b"]).reshape(L, NE),
        "w_gate_up": f(inp["w_gate_up"]).reshape(L * NE * D, 2 * D),
        "b_gate_up": f(inp["b_gate_up"]).reshape(L * NE, 2 * D),
        "w_down": f(inp["w_down"]).reshape(L * NE * D, D),
        "b_down": f(inp["b_down"]).reshape(L * NE, D),
        "consts": make_consts(),
    }


def run(inp, n_cores, S, L, dbg=False):
    nc = build_nc(S, L, dbg)
    shared = pack_inputs(inp, L)
    x = np.asarray(inp["x"], dtype=np.float32)
    in_maps = []
    for c in range(n_cores):
        m = dict(shared)
        m["x"] = np.ascontiguousarray(x[c])
        in_maps.append(m)
    res = run_bass_kernel_spmd(nc, in_maps, core_ids=list(range(n_cores)))
    return res


def kernel(**inputs):
    res = run(inputs, 8, 4096, 4)
    return np.stack([r["out"] for r in res.results], axis=0).astype(np.float32)
```
